# Optimizing a Trainium2 kernel written in Bass

```python
import math
import jax, jax.numpy as jnp
from jax import lax
import numpy as np

D_MODEL = 2048
BATCH = 8
SEQ = 2048
DEPTH = 1

CHUNK = 64
N_META = 16
EPS = 1e-5
ALPHA = (2 * DEPTH) ** 0.25
BETA = (8 * DEPTH) ** -0.25
MIX_WIDTH = D_MODEL
MIX_A = MIX_WIDTH // 2
MIX_B = MIX_WIDTH - MIX_A
A_HEAD_DIM = 128
A_HEADS = MIX_A // A_HEAD_DIM
A_KV_RANK = 256
IDX_HEADS = 16
IDX_DIM = 64
TOPK_MAX = 256
Q_BLOCK = 128
REL_BUCKETS = 32
REL_MAX_DIST = 128
G_HEADS = 4
G_VAL_DIM = MIX_B // G_HEADS
G_KEY_DIM = G_VAL_DIM // 2
G_GATE_RANK = 16
G_GATE_NORM = 16.0
P_HEADS = 8
P_NKEYS = 128
P_NEXPERTS = P_NKEYS * P_NKEYS
P_QDIM = 256
P_TOPK = 16
P_BLOCK = 256
SPLITS = (A_HEADS * A_HEAD_DIM,
          A_KV_RANK,
          IDX_HEADS * IDX_DIM,
          IDX_DIM,
          IDX_HEADS,
          G_HEADS * G_KEY_DIM,
          G_HEADS * G_KEY_DIM,
          G_HEADS * G_VAL_DIM,
          G_GATE_RANK,
          G_HEADS * G_VAL_DIM)
IN_COLS = sum(SPLITS)

kernel_name = 'hybrid_dsa_gla_peer_stream_encoder'


def layer_norm(x, g, b):
    xf = x.astype(jnp.float32)
    mu = jnp.mean(xf, axis=-1, keepdims=True)
    var = jnp.mean(jnp.square(xf - mu), axis=-1, keepdims=True)
    y = (xf - mu) * lax.rsqrt(var + EPS)
    return (y * g.astype(jnp.float32) + b.astype(jnp.float32)).astype(x.dtype)


def rms_norm_f32(x, g):
    xf = x.astype(jnp.float32)
    return xf * lax.rsqrt(jnp.mean(xf * xf, axis=-1, keepdims=True) + EPS) * g.astype(jnp.float32)


def chunk_id(pos):
    return jnp.where(pos < N_META, 0, 1 + (pos - N_META) // CHUNK)


def t5_bucket(rel):
    nb = REL_BUCKETS // 2
    max_exact = nb // 2
    base = jnp.where(rel > 0, nb, 0)
    n = jnp.abs(rel)
    nf = jnp.maximum(n, 1).astype(jnp.float32)
    large = max_exact + (jnp.log(nf / max_exact) / math.log(REL_MAX_DIST / max_exact)
                         * (nb - max_exact)).astype(jnp.int32)
    large = jnp.minimum(large, nb - 1)
    return base + jnp.where(n < max_exact, n, large)


def dsa_mixer(q, ckv, iq, ik, iw, w_uk, w_uv, rel_bias, topk):
    B, L = q.shape[0], q.shape[1]
    ql = jnp.einsum('blhd,hrd->blhr', q, w_uk) * (A_HEAD_DIM ** -0.5)
    iq = iq * (IDX_DIM ** -0.5)
    iw = iw * (IDX_HEADS ** -0.5)
    nblk = -(-L // Q_BLOCK)
    Lp = nblk * Q_BLOCK

    def to_blocks(a):
        a = jnp.pad(a, [(0, 0), (0, Lp - L)] + [(0, 0)] * (a.ndim - 2))
        return jnp.moveaxis(a.reshape((B, nblk, Q_BLOCK) + a.shape[2:]), 1, 0)

    kpos = jnp.arange(L, dtype=jnp.int32)
    kch = chunk_id(kpos)
    qpos = jnp.arange(Lp, dtype=jnp.int32).reshape(nblk, Q_BLOCK)

    def one_block(args):
        iq_b, iw_b, ql_b, qp = args
        adm = kch[None, :] <= chunk_id(qp)[:, None]
        logit = jnp.einsum('bqhd,bkd->bqhk', iq_b, ik)
        score = jnp.einsum('bqhk,bqh->bqk', jax.nn.relu(logit), iw_b).astype(jnp.float32)
        score = jnp.where(adm[None], score, -jnp.inf)
        _, sel = lax.top_k(score, topk)
        valid = jnp.take_along_axis(jnp.broadcast_to(adm[None], score.shape), sel, axis=-1)
        kv = jax.vmap(lambda c, s: c[s])(ckv, sel)
        bias = rel_bias[t5_bucket(kpos[sel] - qp[None, :, None])]
        logits = (jnp.einsum('bqhr,bqkr->bqhk', ql_b, kv).astype(jnp.float32)
                  + jnp.swapaxes(bias, -1, -2).astype(jnp.float32))
        logits = jnp.where(valid[:, :, None, :], logits, -jnp.inf)
        p = jax.nn.softmax(logits, axis=-1).astype(kv.dtype)
        return jnp.einsum('bqhk,bqkr->bqhr', p, kv)

    ol = lax.map(one_block, (to_blocks(iq), to_blocks(iw), to_blocks(ql), qpos))
    ol = jnp.moveaxis(ol, 0, 1).reshape(B, Lp, A_HEADS, A_KV_RANK)[:, :L]
    o = jnp.einsum('blhr,hrd->blhd', ol, w_uv)
    return o.reshape(B, L, A_HEADS * A_HEAD_DIM)


def gla_mixer(q, k, v, gk, og, norm_g):
    B, L = q.shape[0], q.shape[1]
    lead = CHUNK - N_META
    nC = (L + lead) // CHUNK

    def to_chunks(a):
        a = jnp.pad(a.astype(jnp.float32), [(0, 0), (lead, 0)] + [(0, 0)] * (a.ndim - 2))
        return jnp.moveaxis(a.reshape((B, nC, CHUNK) + a.shape[2:]), 1, 0)

    causal = jnp.tril(jnp.ones((CHUNK, CHUNK), dtype=bool))

    def step(S, inp):
        qc, kc, vc, gc = inp
        b = jnp.cumsum(gc, axis=1)
        o_inter = jnp.einsum('bchd,bhdv->bchv', qc * jnp.exp(b), S)
        diff = b[:, :, None] - b[:, None, :]
        decay = jnp.where(causal[None, :, :, None, None], jnp.exp(jnp.minimum(diff, 0.0)), 0.0)
        A = jnp.einsum('bihd,bjhd,bijhd->bhij', qc, kc, decay)
        o_intra = jnp.einsum('bhij,bjhv->bihv', A, vc)
        b_last = b[:, -1]
        S_new = (jnp.exp(b_last)[..., None] * S
                 + jnp.einsum('bjhd,bjhv->bhdv', kc * jnp.exp(b_last[:, None] - b), vc))
        return S_new, o_inter + o_intra

    S0 = jnp.zeros((B, G_HEADS, G_KEY_DIM, G_VAL_DIM), jnp.float32)
    qs = q * (G_KEY_DIM ** -0.5)
    _, o = lax.scan(step, S0, (to_chunks(qs), to_chunks(k), to_chunks(v), to_chunks(gk)))
    o = jnp.moveaxis(o, 0, 1).reshape(B, nC * CHUNK, G_HEADS, G_VAL_DIM)[:, lead:]
    o = rms_norm_f32(o, norm_g).astype(og.dtype).reshape(B, L, G_HEADS * G_VAL_DIM)
    return o * jax.nn.silu(og)


def peer_ffn(x, w_pq, sub_keys, u_tab, v_tab):
    B, L, D = x.shape
    T = B * L
    nblk = -(-T // P_BLOCK)
    xt = jnp.pad(x.reshape(T, D), ((0, nblk * P_BLOCK - T), (0, 0))).reshape(nblk, P_BLOCK, D)

    def one_block(xb):
        qh = (xb @ w_pq).reshape(P_BLOCK, P_HEADS, 2, P_QDIM // 2)
        s = jnp.einsum('thcd,hcnd->thcn', qh, sub_keys).astype(jnp.float32)
        s1, i1 = lax.top_k(s[:, :, 0], P_TOPK)
        s2, i2 = lax.top_k(s[:, :, 1], P_TOPK)
        cand = (s1[..., :, None] + s2[..., None, :]).reshape(P_BLOCK, P_HEADS, P_TOPK * P_TOPK)
        cidx = (i1[..., :, None] * P_NKEYS + i2[..., None, :]).reshape(P_BLOCK, P_HEADS, P_TOPK * P_TOPK)
        top, pos = lax.top_k(cand, P_TOPK)
        eidx = jnp.take_along_axis(cidx, pos, axis=-1).reshape(P_BLOCK, P_HEADS * P_TOPK)
        g = jax.nn.softmax(top, axis=-1).reshape(P_BLOCK, P_HEADS * P_TOPK).astype(xb.dtype)
        act = jax.nn.gelu(jnp.einsum('td,tkd->tk', xb, u_tab[eidx]))
        return jnp.einsum('tk,tkd->td', g * act, v_tab[eidx])

    y = lax.map(one_block, xt).reshape(nblk * P_BLOCK, D)[:T]
    return y.reshape(B, L, D)


def setup_inputs(seed: int = 0) -> dict:
    key = jax.random.key(seed)
    ks = jax.random.split(key, 20)
    f32 = jnp.float32

    def nrm(k, shape, scale):
        return jax.random.normal(k, shape, f32) * scale

    return {
        'x': nrm(ks[0], (BATCH, SEQ, D_MODEL), 1.0),
        'meta_tokens': nrm(ks[1], (N_META, D_MODEL), 1.0),
        'ln0_g': 1.0 + nrm(ks[2], (D_MODEL,), 0.02),
        'ln0_b': nrm(ks[3], (D_MODEL,), 0.02),
        'rel_bias': nrm(ks[4], (REL_BUCKETS, A_HEADS), 0.5),
        'w_in': nrm(ks[5], (DEPTH, D_MODEL, IN_COLS), D_MODEL ** -0.5),
        'w_uk': nrm(ks[6], (DEPTH, A_HEADS, A_KV_RANK, A_HEAD_DIM), A_HEAD_DIM ** -0.5),
        'w_uv': nrm(ks[7], (DEPTH, A_HEADS, A_KV_RANK, A_HEAD_DIM), A_KV_RANK ** -0.5),
        'w_gk2': nrm(ks[8], (DEPTH, G_GATE_RANK, G_HEADS * G_KEY_DIM), G_GATE_RANK ** -0.5),
        'b_gk': nrm(ks[9], (DEPTH, G_HEADS * G_KEY_DIM), 0.1),
        'gla_norm_g': 1.0 + nrm(ks[10], (DEPTH, G_VAL_DIM), 0.02),
        'w_out': nrm(ks[11], (DEPTH, MIX_WIDTH, D_MODEL), BETA * MIX_WIDTH ** -0.5),
        'ln1_g': 1.0 + nrm(ks[12], (DEPTH, D_MODEL), 0.02),
        'ln1_b': nrm(ks[13], (DEPTH, D_MODEL), 0.02),
        'w_pq': nrm(ks[14], (DEPTH, D_MODEL, P_HEADS * P_QDIM), D_MODEL ** -0.5),
        'sub_keys': nrm(ks[15], (DEPTH, P_HEADS, 2, P_NKEYS, P_QDIM // 2), (P_QDIM // 2) ** -0.5),
        'u_tab': nrm(ks[16], (DEPTH, P_NEXPERTS, D_MODEL), D_MODEL ** -0.5),
        'v_tab': nrm(ks[17], (DEPTH, P_NEXPERTS, D_MODEL), BETA * P_HEADS ** -0.5),
        'ln2_g': 1.0 + nrm(ks[18], (DEPTH, D_MODEL), 0.02),
        'ln2_b': nrm(ks[19], (DEPTH, D_MODEL), 0.02),
    }


def reference(x, meta_tokens, ln0_g, ln0_b, rel_bias, w_in, w_uk, w_uv, w_gk2, b_gk,
              gla_norm_g, w_out, ln1_g, ln1_b, w_pq, sub_keys, u_tab, v_tab, ln2_g, ln2_b):
    B, S, D = x.shape
    L = S + N_META
    topk = min(TOPK_MAX, S // 4)
    offsets = []
    acc = 0
    for w in SPLITS[:-1]:
        acc += w
        offsets.append(acc)
    meta = jnp.broadcast_to(meta_tokens[None].astype(x.dtype), (B, N_META, D))
    h = layer_norm(jnp.concatenate([meta, x], axis=1), ln0_g, ln0_b)
    for l in range(DEPTH):
        proj = h @ w_in[l]
        a_q, a_ckv, i_q, i_k, i_w, g_q, g_k, g_v, g_r, g_o = jnp.split(proj, offsets, axis=-1)
        y_a = dsa_mixer(a_q.reshape(B, L, A_HEADS, A_HEAD_DIM), a_ckv,
                        i_q.reshape(B, L, IDX_HEADS, IDX_DIM), i_k, i_w,
                        w_uk[l], w_uv[l], rel_bias, topk)
        gk = jax.nn.log_sigmoid((g_r @ w_gk2[l] + b_gk[l]).astype(jnp.float32)) / G_GATE_NORM
        y_b = gla_mixer(g_q.reshape(B, L, G_HEADS, G_KEY_DIM), g_k.reshape(B, L, G_HEADS, G_KEY_DIM),
                        g_v.reshape(B, L, G_HEADS, G_VAL_DIM), gk.reshape(B, L, G_HEADS, G_KEY_DIM),
                        g_o, gla_norm_g[l])
        mix = jnp.concatenate([y_a, y_b], axis=-1) @ w_out[l]
        h = layer_norm(ALPHA * h + mix, ln1_g[l], ln1_b[l])
        h = layer_norm(ALPHA * h + peer_ffn(h, w_pq[l], sub_keys[l], u_tab[l], v_tab[l]), ln2_g[l], ln2_b[l])
    return h[:, N_META:]
```

```python
import contextlib
import math
import numpy as np
import ml_dtypes
import concourse.bass as bass
import concourse.mybir as mybir
from concourse.bass_utils import run_bass_kernel_spmd

F32 = mybir.dt.float32
BF = mybir.dt.bfloat16
ALU = mybir.AluOpType
AF = mybir.ActivationFunctionType
AX = mybir.AxisListType

D = 2048
S = 2048
NM = 16
L = S + NM
NT = S // 128
EPS = 1e-5
ALPHA = 2.0 ** 0.25
IN_COLS = 5472
O_AQ, O_CKV, O_IQ, O_IK, O_IW, O_GQ, O_GK, O_GV, O_GR, O_GO = (
    0, 1024, 1280, 2304, 2368, 2384, 2896, 3408, 4432, 4448)
NEG = -1.0e30


class Trk:
    def __init__(self, h, name, sb):
        self.h = h
        self.name = name
        self.sb = sb
        self.w = {}
        self.r = {}
        self.dsem = None

    def __getitem__(self, k):
        return V(self, self.h[k])

    @property
    def v(self):
        return V(self, self.h.ap() if hasattr(self.h, "ap") else self.h[:])


class V:
    def __init__(self, o, ap):
        self.o = o
        self.ap = ap

    def __getitem__(self, k):
        return V(self.o, self.ap[k])

    def bitcast(self, dt):
        return V(self.o, self.ap.bitcast(dt))

    def rearrange(self, pattern_, **kw):
        return V(self.o, self.ap.rearrange(pattern_, **kw))

    def bc(self, shape):
        return V(self.o, self.ap.to_broadcast(shape))


class Prog:
    def __init__(self, nc, es):
        self.nc = nc
        self.es = es
        self.e = dict(pe=nc.tensor, act=nc.scalar, dve=nc.vector, pool=nc.gpsimd, sp=nc.sync)
        self.sem = {}
        self.cnt = {}
        for k in ("pe", "act", "dve", "pool"):
            self.sem[k] = es.enter_context(nc.semaphore("S_" + k))
            self.cnt[k] = 0
        self.waited = {k: {} for k in self.e}
        self.dfree = []
        self.dfree_sw = []
        self.ndsem = 0
        self.bank_i = 0
        self.n_inst = 0

    def sb(self, stack, name, shape, dt):
        self.n_sb = getattr(self, "n_sb", 0) + 1
        name = "%s_%d" % (name, self.n_sb)
        h = stack.enter_context(self.nc.sbuf_tensor(name, list(shape), dt))
        t = Trk(h, name, True)
        if hasattr(stack, "tiles"):
            stack.tiles.append(t)
        return t

    def dram(self, name, shape, dt, kind=None):
        if kind:
            h = self.nc.dram_tensor(name, list(shape), dt, kind=kind)
        else:
            h = self.nc.dram_tensor(name, list(shape), dt)
        return Trk(h, name, False)

    def _get_dsem(self, t, sw):
        if t.dsem is None:
            t.dsem = {}
        if sw not in t.dsem:
            free = self.dfree_sw if sw else self.dfree
            if free:
                t.dsem[sw] = free.pop()
            else:
                self.ndsem += 1
                key = ("W%d" if sw else "D%d") % self.ndsem
                self.sem[key] = self.es.enter_context(self.nc.semaphore(key))
                self.cnt[key] = 0
                t.dsem[sw] = key
        return t.dsem[sw]

    @contextlib.contextmanager
    def scope(self):
        st = contextlib.ExitStack()
        st.tiles = []
        try:
            yield st
        finally:
            self.barrier()
            self.release(*st.tiles)
            st.close()

    def release(self, *ts):
        for t in ts:
            if t.dsem is not None:
                for sw, key in t.dsem.items():
                    (self.dfree_sw if sw else self.dfree).append(key)
                t.dsem = None

    def _wait(self, eng, key, val):
        if self.waited[eng].get(key, 0) >= val:
            return
        self.e[eng].wait_ge(self.sem[key], val)
        self.waited[eng][key] = val

    def _sync(self, eng, reads, writes):
        need = {}
        for t in reads:
            for k, v in t.w.items():
                if need.get(k, 0) < v:
                    need[k] = v
        for t in writes:
            for k, v in t.w.items():
                if need.get(k, 0) < v:
                    need[k] = v
            for k, v in t.r.items():
                if need.get(k, 0) < v:
                    need[k] = v
        for k, v in need.items():
            if k == eng and eng == "pe":
                continue
            self._wait(eng, k, v)

    def _done(self, key, val, reads, writes):
        for t in reads:
            t.r[key] = val
        for t in writes:
            t.w = {key: val}
            t.r = {}

    def op(self, eng, reads, writes, fn):
        reads = [x.o for x in reads if isinstance(x, V)]
        writes = [x.o for x in writes]
        self._sync(eng, reads, writes)
        ins = fn(self.e[eng])
        self.cnt[eng] += 1
        ins.then_inc(self.sem[eng], 1)
        self._done(eng, self.cnt[eng], reads, writes)
        self.n_inst += 1

    def dma(self, q, out, in_, **kw):
        sbt = out.o if out.o.sb else in_.o
        self._sync(q, [in_.o], [out.o])
        ins = self.e[q].dma_start(out=out.ap, in_=in_.ap, **kw)
        key = self._get_dsem(sbt, q == "pool")
        self.cnt[key] += 16
        ins.then_inc(self.sem[key], 16)
        self._done(key, self.cnt[key], [in_.o], [out.o])
        self.n_inst += 1

    def barrier(self, engines=("pe", "act", "dve", "pool", "sp")):
        for e in engines:
            for k, v in self.cnt.items():
                if v > 0 and k != e:
                    self._wait(e, k, v)

    def mm(self, out, lhsT, rhs, start=True, stop=True):
        self.op("pe", [lhsT, rhs], [out],
                lambda e: e.matmul(out.ap, lhsT.ap, rhs.ap, start=start, stop=stop))

    def tr(self, out, in_, ident):
        self.op("pe", [in_, ident], [out], lambda e: e.transpose(out.ap, in_.ap, ident.ap))

    def act(self, out, in_, func, bias=0.0, scale=1.0, accum=None, eng="act"):
        rd = [in_, bias, scale]
        wr = [out] + ([accum] if accum is not None else [])
        b = bias.ap if isinstance(bias, V) else bias
        sc = scale.ap if isinstance(scale, V) else scale
        kw = {}
        if accum is not None:
            kw["accum_out"] = accum.ap
        self.op("act", rd, wr, lambda e: e.activation(out.ap, in_.ap, func, bias=b, scale=sc, **kw))

    def tt(self, eng, out, in0, in1, op):
        self.op(eng, [in0, in1], [out], lambda e: e.tensor_tensor(out.ap, in0.ap, in1.ap, op))

    def ts(self, eng, out, in0, s1, s2, op0, op1=None, accum=None):
        a1 = s1.ap if isinstance(s1, V) else s1
        a2 = s2.ap if isinstance(s2, V) else s2
        kw = {}
        if op1 is not None:
            kw["op1"] = op1
        if accum is not None:
            kw["accum_out"] = accum.ap
        wr = [out] + ([accum] if accum is not None else [])
        self.op(eng, [in0, s1, s2], wr,
                lambda e: e.tensor_scalar(out.ap, in0.ap, a1, a2, op0, **kw))

    def stt(self, eng, out, in0, sc, in1, op0, op1):
        a = sc.ap if isinstance(sc, V) else sc
        self.op(eng, [in0, sc, in1], [out],
                lambda e: e.scalar_tensor_tensor(out.ap, in0.ap, a, in1.ap, op0, op1))

    def copy(self, eng, out, in_):
        if eng == "act":
            self.op(eng, [in_], [out], lambda e: e.copy(out.ap, in_.ap))
        else:
            self.op(eng, [in_], [out], lambda e: e.tensor_copy(out.ap, in_.ap))

    def memset(self, eng, out, val):
        self.op(eng, [], [out], lambda e: e.memset(out.ap, val))

    def rsum(self, eng, out, in_):
        self.op(eng, [in_], [out], lambda e: e.reduce_sum(out.ap, in_.ap, AX.X))

    def recip(self, out, in_):
        self.op("dve", [in_], [out], lambda e: e.reciprocal(out.ap, in_.ap))

    def max8(self, out, in_):
        self.op("dve", [in_], [out], lambda e: e.max(out=out.ap, in_=in_.ap))

    def mrep(self, out, rep, vals, imm):
        self.op("dve", [rep, vals], [out],
                lambda e: e.match_replace(out=out.ap, in_to_replace=rep.ap, in_values=vals.ap,
                                          imm_value=imm))


def build(debug=None, stop_after=None):
    nc = bass.Bass("TRN2", target_bir_lowering=False)
    es = contextlib.ExitStack()
    with es:
        p = Prog(nc, es)
        IN = {}

        def inp(name, shape, dt=F32):
            IN[name] = p.dram(name, shape, dt, kind="ExternalInput")
            return IN[name]

        x = inp("x", [S, D])
        meta = inp("meta_tokens", [NM, D])
        ln0_g = inp("ln0_g", [1, D]); ln0_b = inp("ln0_b", [1, D])
        rel_bias = inp("rel_bias", [32, 8])
        w_in = inp("w_in", [D, IN_COLS])
        w_uk = inp("w_uk", [8, 256, 128]); w_uv = inp("w_uv", [8, 256, 128])
        w_gk2 = inp("w_gk2", [16, 512]); b_gk = inp("b_gk", [1, 512])
        gla_g = inp("gla_norm_g", [1, 256])
        w_out = inp("w_out", [D, D])
        ln1_g = inp("ln1_g", [1, D]); ln1_b = inp("ln1_b", [1, D])
        w_pq = inp("w_pq", [D, D])
        sub_keys = inp("sub_keys", [16, 128, 128])
        u_tab = inp("u_tab", [16384, D]); v_tab = inp("v_tab", [16384, D])
        ln2_g = inp("ln2_g", [1, D]); ln2_b = inp("ln2_b", [1, D])
        ident_in = inp("c_ident", [128, 128])
        tri_in = inp("c_tri", [128, 128])
        boh_in = inp("c_boh", [32, 512])
        jrev_in = inp("c_jrev", [128, 128])
        out_d = p.dram("out", [S, D], F32, kind="ExternalOutput")

        dbg = {}

        def scratch(name, shape, dt):
            kind = "ExternalOutput" if (debug and name in debug) else None
            t = p.dram(name, shape, dt, kind=kind)
            dbg[name] = t
            return t

        blk = es.enter_context(nc.Block())
        gs = contextlib.ExitStack()
        es.enter_context(gs)
        banks = []
        for i in range(8):
            h = es.enter_context(nc.psum_tensor("bank%d" % i, [128, 512], F32))
            banks.append(Trk(h, "bank%d" % i, True))

        def bank():
            b = banks[p.bank_i % 8]
            p.bank_i += 1
            return b

        ident_f = p.sb(gs, "ident_f", [128, 128], F32)
        ident_b = p.sb(gs, "ident_b", [128, 128], BF)
        tri_f = p.sb(gs, "tri_f", [128, 128], F32)
        tri_b = p.sb(gs, "tri_b", [128, 128], BF)
        p.dma("sp", ident_f.v, ident_in.v)
        p.dma("sp", tri_f.v, tri_in.v)
        p.copy("dve", ident_b.v, ident_f.v)
        p.copy("dve", tri_b.v, tri_f.v)
        jrev_b = p.sb(gs, "jrev_b", [128, 128], BF)
        p.dma("pool", jrev_b.v, jrev_in.v)

        def layer_norm(n, src, dst, gB, bB, st, junk):
            p.rsum("dve", st[:n, 0:1], src)
            p.ts("dve", st[:n, 1:2], st[:n, 0:1], -1.0 / D, None, ALU.mult)
            p.act(src, src, AF.Identity, bias=st[:n, 1:2])
            p.memset("dve", st[:n, 2:3], 0.0)
            p.act(junk, src, AF.Square, accum=st[:n, 2:3])
            p.ts("dve", st[:n, 3:4], st[:n, 2:3], 1.0 / D, EPS, ALU.mult, ALU.add)
            p.act(st[:n, 5:6], st[:n, 3:4], AF.Sqrt)
            p.recip(st[:n, 4:5], st[:n, 5:6])
            p.stt("dve", src, src, st[:n, 4:5], gB, ALU.mult, ALU.mult)
            p.tt("dve", src, src, bB, ALU.add)
            p.copy("act", dst, src)

        FT = {}
        for nm, rows in (("qlT", 2048), ("ckvT", 256), ("iqT", 1024), ("ikT", 128),
                         ("gqT", 512), ("gkT", 512), ("grT", 16)):
            FT[nm] = scratch(nm, [rows, L], BF)
        TM = {}
        TM["ckv"] = scratch("ckv", [L, 256], BF)
        TM["iw"] = scratch("iw", [L, 16], F32)
        TM["gv"] = scratch("gv", [L, 1024], BF)
        TM["go"] = scratch("go", [L, 1024], BF)
        hd = scratch("hd", [S, D], F32)
        h1d = scratch("h1d", [S, D], F32)

        sA = contextlib.ExitStack()
        sA.tiles = []
        hTm = p.sb(sA, "hTm", [128, 16, NM], BF)
        hTb = [p.sb(sA, "hTb%d" % i, [128, 16, 512], BF) for i in range(4)]

        def hT_cols(c0, n):
            if c0 < NM:
                assert c0 + n <= NM
                return hTm, slice(c0, c0 + n)
            b = (c0 - NM) // 512
            o = (c0 - NM) % 512
            assert o + n <= 512
            return hTb[b], slice(o, o + n)
        if True:
            s0 = sA
            gB = p.sb(s0, "gB", [128, D], F32)
            bB = p.sb(s0, "bB", [128, D], F32)
            p.dma("sp", gB.v, V(ln0_g, ln0_g.h.ap().partition_broadcast(128)))
            p.dma("sp", bB.v, V(ln0_b, ln0_b.h.ap().partition_broadcast(128)))
            xts = [p.sb(s0, "xt%d" % i, [128, D], F32) for i in range(2)]
            zts = [p.sb(s0, "zt%d" % i, [128, D], BF) for i in range(2)]
            junks = [p.sb(s0, "junk%d" % i, [128, D], BF) for i in range(2)]
            sts0 = [p.sb(s0, "st%d" % i, [128, 8], F32) for i in range(2)]
            for ti in range(NT + 1):
                n = NM if ti == 0 else 128
                c0 = 0 if ti == 0 else NM + (ti - 1) * 128
                xt = xts[ti % 2]; zt = zts[ti % 2]
                if ti == 0:
                    p.dma("sp", xt[:n, :], meta.v)
                else:
                    p.dma("sp", xt[:n, :], x[(ti - 1) * 128: ti * 128, :])
                layer_norm(n, xt[:n, :], zt[:n, :], gB[:n, :], bB[:n, :], sts0[ti % 2], junks[ti % 2][:n, :])
                if ti > 0:
                    p.dma("sp", hd[(ti - 1) * 128: ti * 128, :], xt.v)
                for half in range(2):
                    bk = bank()
                    bv = bk.v.bitcast(BF)
                    for c in range(8):
                        cc = half * 8 + c
                        p.tr(bv[:, c * 128: c * 128 + n], zt[:n, cc * 128:(cc + 1) * 128],
                             ident_b[:n, :n])
                    src = bv.rearrange("p (c t) -> p c t", t=128)[:, :, :n]
                    ht_, sl_ = hT_cols(c0, n)
                    p.copy("act", ht_[:, half * 8:(half + 1) * 8, sl_], src)
        with p.scope() as s1:
            wsl = [p.sb(s1, "wsl%d" % i, [128, 16, 128], BF) for i in range(3)]
            stg = [p.sb(s1, "stg%d" % i, [128, 512], BF) for i in range(4)]
            stgf = [p.sb(s1, "stgf%d" % i, [128, 16], F32) for i in range(2)]
            wukT = p.sb(s1, "wukT", [128, 8, 256], BF)
            w_in_r = w_in.h.ap().rearrange("(c p) n -> p c n", p=128)
            with p.scope() as s2:
                wuk_n = p.sb(s2, "wuk_n", [128, 16, 128], BF)
                p.dma("pool", wuk_n.v, V(w_uk, w_uk.h.ap().rearrange("h (rc p) d -> p (h rc) d", p=128)))
                for g in range(2):
                    bk = bank(); bv = bk.v.bitcast(BF)
                    for j in range(8):
                        hr = g * 8 + j
                        p.tr(bv[:, j * 128:(j + 1) * 128], wuk_n[:, hr, :], ident_b.v)
                    p.copy("dve", wukT[:, g * 4:(g + 1) * 4, :].rearrange("p h r -> p (h r)"), bv)
            wi = [0]
            si = [0]

            def load_w(col0, ncols, dup=False):
                w = wsl[wi[0] % 3]; wi[0] += 1
                if dup:
                    p.dma("pool", w[:, :, 0:ncols], V(w_in, w_in_r[:, :, col0:col0 + ncols]))
                    p.dma("pool", w[:, :, ncols:2 * ncols], V(w_in, w_in_r[:, :, col0:col0 + ncols]))
                else:
                    p.dma("pool", w[:, :, 0:ncols], V(w_in, w_in_r[:, :, col0:col0 + ncols]))
                return w

            tok_blocks = [(NM + i * 512, 512) for i in range(4)] + [(0, NM)]

            def fm_group(col0, rows, dst, dst_row0, real_only=False, dup=False, post=None):
                w = load_w(col0, rows // 2 if dup else rows, dup)
                for (t0, tn) in tok_blocks:
                    if real_only and t0 == 0:
                        continue
                    bk = bank()
                    ht_, sl_ = hT_cols(t0, tn)
                    for c in range(16):
                        p.mm(bk[:rows, :tn], w[:, c, :rows], ht_[:, c, sl_],
                             start=(c == 0), stop=(c == 15))
                    sg = stg[si[0] % 4]; si[0] += 1
                    if si[0] % 2:
                        p.copy("act", sg[:rows, :tn], bk[:rows, :tn])
                    else:
                        p.copy("dve", sg[:rows, :tn], bk[:rows, :tn])
                    if post is not None:
                        post(sg, t0, tn)
                    else:
                        p.dma("sp", dst[dst_row0:dst_row0 + rows, t0:t0 + tn], sg[:rows, :tn])

            for h in range(8):
                def post(sg, t0, tn, h=h):
                    for rc in range(2):
                        bk = bank()
                        p.mm(bk[:, :tn], wukT[:, h, rc * 128:(rc + 1) * 128], sg[:, :tn])
                        s2_ = stg[si[0] % 4]; si[0] += 1
                        p.act(s2_[:, :tn], bk[:, :tn], AF.Copy, scale=128.0 ** -0.5)
                        r0 = (h * 2 + rc) * 128
                        p.dma("sp", FT["qlT"][r0:r0 + 128, t0:t0 + tn], s2_[:, :tn])
                fm_group(O_AQ + h * 128, 128, None, 0, real_only=True, post=post)
            for c in range(2):
                fm_group(O_CKV + c * 128, 128, FT["ckvT"], c * 128)
            for c in range(8):
                fm_group(O_IQ + c * 128, 128, FT["iqT"], c * 128, real_only=True)
            fm_group(O_IK, 128, FT["ikT"], 0, dup=True)
            for c in range(4):
                fm_group(O_GQ + c * 128, 128, FT["gqT"], c * 128, real_only=True)
            for c in range(4):
                fm_group(O_GK + c * 128, 128, FT["gkT"], c * 128)
            fm_group(O_GR, 16, FT["grT"], 0)

            with p.scope() as s3:
                wtm = [p.sb(s3, "wtm%d" % i, [128, 16, 512], BF) for i in range(2)]
                k = 0
                for (col0, ncols, dst, dcol0, real_only) in (
                        (O_CKV, 256, TM["ckv"], 0, False),
                        (O_IW, 16, TM["iw"], 0, True),
                        (O_GV, 512, TM["gv"], 0, False), (O_GV + 512, 512, TM["gv"], 512, False),
                        (O_GO, 512, TM["go"], 0, True), (O_GO + 512, 512, TM["go"], 512, True)):
                    w = wtm[k % 2]; k += 1
                    p.dma("pool", w[:, :, :ncols], V(w_in, w_in_r[:, :, col0:col0 + ncols]))
                    for ti in range(NT + 1):
                        if real_only and ti == 0:
                            continue
                        n = NM if ti == 0 else 128
                        c0 = 0 if ti == 0 else NM + (ti - 1) * 128
                        bk = bank()
                        ht_, sl_ = hT_cols(c0, n)
                        for c in range(16):
                            p.mm(bk[:n, :ncols], ht_[:, c, sl_], w[:, c, :ncols],
                                 start=(c == 0), stop=(c == 15))
                        if dst is TM["iw"]:
                            sg = stgf[si[0] % 2]; si[0] += 1
                            p.copy("dve", sg[:n, :ncols], bk[:n, :ncols])
                        else:
                            sg = stg[si[0] % 4]; si[0] += 1
                            if si[0] % 2:
                                p.copy("act", sg[:n, :ncols], bk[:n, :ncols])
                            else:
                                p.copy("dve", sg[:n, :ncols], bk[:n, :ncols])
                        p.dma("sp", dst[c0:c0 + n, dcol0:dcol0 + ncols], sg[:n, :ncols])
        p.barrier()
        p.release(*sA.tiles)
        sA.close()

        if stop_after == "B":
            p.barrier()
            return nc, dbg

        def bank6():
            b = banks[p.bank_i % 6]
            p.bank_i += 1
            return b
        ob_i = [0]

        def obank():
            ob_i[0] += 1
            return banks[6 + ob_i[0] % 2]

        sY = contextlib.ExitStack()
        sY.tiles = []
        es.enter_context(sY)
        yT = p.sb(sY, "yT", [128, 16, S], BF)
        yTd = scratch("yTd", [128, 16 * S], BF)

        Etab = scratch("Etab", [8, 512], F32)
        if debug and "scoreD" in debug:
            scratch("scoreD", [128, L], F32)
            scratch("thrD", [128, 1], F32)
            scratch("olD", [128, 2048], BF)
            scratch("maskTD", [128, 17 * 128], BF)
        with p.scope() as sc:
            ikT = p.sb(sc, "ikT_s", [128, L], BF)
            ckvT = p.sb(sc, "ckvT_s", [128, 2, L], BF)
            ckva = p.sb(sc, "ckva", [128, 17, 257], BF)
            wuv = p.sb(sc, "wuv", [128, 16, 128], BF)
            BT = p.sb(sc, "BT", [128, 8, 4, 128], BF)
            p.dma("sp", ikT.v, FT["ikT"].v)
            p.dma("sp", ckvT.v, V(FT["ckvT"], FT["ckvT"].h.ap().rearrange("(c p) t -> p c t", p=128)))
            p.memset("dve", ckva.v, 1.0)
            p.dma("sp", ckva[:NM, 0, 0:256], TM["ckv"][0:NM, :])
            p.dma("sp", ckva[:, 1:17, 0:256],
                  V(TM["ckv"], TM["ckv"].h[NM:L, :].rearrange("(t p) r -> p t r", p=128)))
            p.dma("pool", wuv.v, V(w_uv, w_uv.h.ap().rearrange("h (rc p) d -> p (h rc) d", p=128)))
            with p.scope() as sb_:
                rb = p.sb(sb_, "rb", [32, 8], F32)
                boh = p.sb(sb_, "boh", [32, 512], F32)
                es_ = p.sb(sb_, "es_", [8, 512], F32)
                p.dma("sp", rb.v, rel_bias.v)
                p.dma("sp", boh.v, boh_in.v)
                bk = bank6()
                p.mm(bk[:8, :], rb.v, boh.v)
                p.copy("dve", es_.v, bk[:8, :])
                p.dma("sp", Etab.v, es_.v)
            for h in range(8):
                for kind, (off, ncol, ps) in enumerate(((128, 128, 1), (0, 128, 1), (0, 128, 0), (112, NM, 1))):
                    src = bass.AP(tensor=Etab.h, offset=h * 512 + off, ap=[[ps, 128], [1, ncol]])
                    p.dma("pool", BT[:, h, kind, :ncol], V(Etab, src))

            iqs = [p.sb(sc, "iqs%d" % i, [128, 8, 128], BF) for i in range(2)]
            qls = [p.sb(sc, "qls%d" % i, [128, 16, 128], BF) for i in range(3)]
            iws = [p.sb(sc, "iws%d" % i, [128, 16], F32) for i in range(2)]
            scores_ = [p.sb(sc, "score%d" % i, [128, L], F32) for i in range(2)]
            rl = [p.sb(sc, "rl%d" % i, [128, 512], F32) for i in range(4)]
            NB = 20
            ck = p.sb(sc, "ck", [128, NB], F32)
            for k in range(NB):
                p.memset("pool", ck[:, k:k + 1], 0.5 ** (k + 1))
            bsts = [p.sb(sc, "bst%d" % i, [128, 8], F32) for i in range(2)]
            nRks = [p.sb(sc, "nRk%d" % i, [128, NB], F32) for i in range(2)]
            cnts = [p.sb(sc, "cnt%d" % i, [128, NB], F32) for i in range(2)]
            sjunk = p.sb(sc, "sjunk", [128, L], BF)
            thr = p.sb(sc, "thr", [128, 1], F32)
            maskq = p.sb(sc, "maskq", [128, L], BF)
            maskTs = [p.sb(sc, "maskT%d" % i, [128, 17, 128], BF) for i in range(2)]
            PTs = [p.sb(sc, "PT%d" % i, [128, 512], BF) for i in range(4)]
            rden = p.sb(sc, "rden", [128, 16], F32)
            olsb = p.sb(sc, "olsb", [128, 8, 256], BF)
            olT = p.sb(sc, "olT", [128, 16, 128], BF)
            iqT_r = FT["iqT"].h.ap().rearrange("(c p) t -> p c t", p=128)
            qlT_r = FT["qlT"].h.ap().rearrange("(c p) t -> p c t", p=128)
            rli = [0]; pti = [0]

            def tiles_of(tq):
                return [(0, 0, NM)] + [(i + 1, NM + 128 * i, 128) for i in range(tq + 1)]

            def bisect_steps(tq):
                Wk = NM + 128 * (tq + 1)
                if Wk <= 256:
                    return
                score = scores_[tq % 2]; bst = bsts[tq % 2]; nRk = nRks[tq % 2]; cnt = cnts[tq % 2]
                for k in range(NB):
                    p.ts("dve", sjunk[:, :Wk], score[:, :Wk], bst[:, 4:5], 0.0, ALU.is_ge, ALU.add,
                         accum=cnt[:, k:k + 1])
                    p.stt("dve", bst[:, 5:6], cnt[:, k:k + 1], 256.0, nRk[:, k:k + 1], ALU.is_ge, ALU.mult)
                    kn_ = k + 1 if k + 1 < NB else k
                    p.stt("dve", bst[:, 4:5], bst[:, 4:5], nRk[:, kn_:kn_ + 1], bst[:, 5:6], ALU.subtract, ALU.add)
                    yield

            def scores(tq, steps):
                q0 = NM + tq * 128
                Wk = NM + 128 * (tq + 1)
                iq = iqs[tq % 2]; ql = qls[tq % 3]; iw = iws[tq % 2]
                score = scores_[tq % 2]; bst = bsts[tq % 2]; nRk = nRks[tq % 2]; cnt = cnts[tq % 2]
                p.dma("sp", iq.v, V(FT["iqT"], iqT_r[:, :, q0:q0 + 128]))
                p.dma("sp", ql.v, V(FT["qlT"], qlT_r[:, :, q0:q0 + 128]))
                p.dma("sp", iw.v, TM["iw"][q0:q0 + 128, :])
                nblk = len(range(0, Wk, 512))
                for bi, k0 in enumerate(range(0, Wk, 512)):
                    kn = min(512, Wk - k0)
                    for h in range(16):
                        bk = bank6()
                        pb = (h % 2) * 64
                        p.mm(bk[:, :kn], iq[pb:pb + 64, h // 2, :], ikT[pb:pb + 64, k0:k0 + kn])
                        r = rl[rli[0] % 4]; rli[0] += 1
                        p.act(r[:, :kn], bk[:, :kn], AF.Relu)
                        if h == 0:
                            p.ts("dve", score[:, k0:k0 + kn], r[:, :kn], iw[:, 0:1], None, ALU.mult)
                        else:
                            p.stt("dve", score[:, k0:k0 + kn], r[:, :kn], iw[:, h:h + 1],
                                  score[:, k0:k0 + kn], ALU.mult, ALU.add)
                        if h % 4 == 3 and steps is not None:
                            next(steps, None)
                if steps is not None:
                    for _ in steps:
                        pass
                if Wk > 256:
                    p.op("dve", [score.v], [bst.v],
                         lambda e: e.tensor_reduce(bst[:, 0:1].ap, score[:, :Wk].ap, AX.X, ALU.max))
                    p.op("dve", [score.v], [bst.v],
                         lambda e: e.tensor_reduce(bst[:, 1:2].ap, score[:, :Wk].ap, AX.X, ALU.min))
                    p.tt("dve", bst[:, 2:3], bst[:, 0:1], bst[:, 1:2], ALU.subtract)
                    p.ts("dve", bst[:, 2:3], bst[:, 2:3], 2.0, None, ALU.add)
                    p.ts("dve", nRk.v, ck.v, bst[:, 2:3], None, ALU.mult)
                    p.stt("dve", bst[:, 4:5], bst[:, 1:2], -1.0, nRk[:, 0:1], ALU.add, ALU.add)
                p.memset("dve", score[0:64, Wk - 64:Wk], NEG)

            def finalize(tq):
                Wk = NM + 128 * (tq + 1)
                score = scores_[tq % 2]; bst = bsts[tq % 2]
                if Wk > 256:
                    p.ts("dve", thr.v, bst[:, 4:5], -1.0e29, None, ALU.max)
                else:
                    p.memset("dve", thr.v, -1.0e29)
                p.ts("dve", maskq[:, :Wk], score[:, :Wk], thr[:, 0:1], None, ALU.is_ge)

            def mask_transposes(tq):
                maskT = maskTs[tq % 2]
                tiles = tiles_of(tq)
                for g0 in range(0, len(tiles), 8):
                    grp = tiles[g0:g0 + 8]
                    bk = bank6(); bv = bk.v.bitcast(BF)
                    for j, (ti, c0, kn) in enumerate(grp):
                        p.tr(bv[:kn, j * 128:(j + 1) * 128], maskq[:, c0:c0 + kn], ident_b.v)
                    if grp[0][0] == 0:
                        p.copy("act", maskT[:NM, 0, :], bv[:NM, 0:128])
                        if len(grp) > 1:
                            p.copy("act", maskT[:, 1:len(grp), :].rearrange("p t q -> p (t q)"),
                                   bv[:, 128:128 * len(grp)])
                    else:
                        p.copy("act", maskT[:, grp[0][0]:grp[0][0] + len(grp), :].rearrange("p t q -> p (t q)"),
                               bv[:, 0:128 * len(grp)])

            def attention(tq):
                maskT = maskTs[tq % 2]
                ql = qls[tq % 3]
                tiles = tiles_of(tq)
                pend = []

                def flush(item):
                    h, ob, PT, grp, g0, first_ = item
                    for j, (ti, c0, kn) in enumerate(grp):
                        last = (g0 + j == len(tiles) - 1)
                        p.mm(ob[:, 0:257], PT[:kn, j * 128:(j + 1) * 128], ckva[:kn, ti, :],
                             start=(first_ and j == 0), stop=last)
                    if g0 + len(grp) == len(tiles):
                        p.act(rden[:, 8 + h:9 + h], ob[:, 256:257], AF.Ln)
                        p.act(rden[:, h:h + 1], rden[:, 8 + h:9 + h], AF.Exp, scale=-1.0)
                        p.act(olsb[:, h, :], ob[:, 0:256], AF.Copy, scale=rden[:, h:h + 1])

                for h in range(8):
                    ob = obank()
                    for g0 in range(0, len(tiles), 4):
                        grp = tiles[g0:g0 + 4]
                        bk = bank6()
                        for j, (ti, c0, kn) in enumerate(grp):
                            if ti == 0:
                                kind = 3 if tq == 0 else 2
                            else:
                                dlt = tq - (ti - 1)
                                kind = 0 if dlt == 0 else (1 if dlt == 1 else 2)
                            o_ = bk[:kn, j * 128:(j + 1) * 128]
                            p.mm(o_, ckvT[:, 0, c0:c0 + kn], ql[:, h * 2, :], start=True, stop=False)
                            p.mm(o_, ckvT[:, 1, c0:c0 + kn], ql[:, h * 2 + 1, :], start=False, stop=False)
                            p.mm(o_, BT[:, h, kind, :kn], jrev_b.v, start=False, stop=True)
                        PT = PTs[pti[0] % 4]; pti[0] += 1
                        w_ = 128 * len(grp)
                        if grp[0][0] == 0:
                            p.act(PT[:NM, 0:128], bk[:NM, 0:128], AF.Exp)
                            p.tt("pool", PT[:NM, 0:128], PT[:NM, 0:128], maskT[:NM, 0, :], ALU.mult)
                            if len(grp) > 1:
                                p.act(PT[:, 128:w_], bk[:, 128:w_], AF.Exp)
                                p.tt("pool", PT[:, 128:w_], PT[:, 128:w_],
                                     maskT[:, 1:len(grp), :].rearrange("p t q -> p (t q)"), ALU.mult)
                        else:
                            p.act(PT[:, 0:w_], bk[:, 0:w_], AF.Exp)
                            p.tt("pool", PT[:, 0:w_], PT[:, 0:w_],
                                 maskT[:, grp[0][0]:grp[0][0] + len(grp), :].rearrange("p t q -> p (t q)"),
                                 ALU.mult)
                        pend.append((h, ob, PT, grp, g0, g0 == 0))
                        if len(pend) > 2:
                            flush(pend.pop(0))
                while pend:
                    flush(pend.pop(0))
                for g in range(2):
                    bk = bank6(); bv = bk.v.bitcast(BF)
                    for j in range(8):
                        hr = g * 8 + j
                        p.tr(bv[:, j * 128:(j + 1) * 128], olsb[:, hr // 2, (hr % 2) * 128:(hr % 2 + 1) * 128],
                             ident_b.v)
                    p.copy("act", olT[:, g * 8:(g + 1) * 8, :].rearrange("p c q -> p (c q)"), bv)
                for g in range(2):
                    bk = bank6()
                    for j in range(4):
                        h = g * 4 + j
                        for rc in range(2):
                            p.mm(bk[:, j * 128:(j + 1) * 128], wuv[:, h * 2 + rc, :], olT[:, h * 2 + rc, :],
                                 start=(rc == 0), stop=(rc == 1))
                    p.copy("act", yT[:, g * 4:(g + 1) * 4, tq * 128:(tq + 1) * 128],
                           bk.v.rearrange("p (c q) -> p c q", q=128))

            for it_ in range(NT + 2):
                steps = bisect_steps(it_ - 1) if 1 <= it_ <= NT else None
                if it_ < NT:
                    scores(it_, steps)
                elif steps is not None:
                    for _ in steps:
                        pass
                if 1 <= it_ <= NT:
                    finalize(it_ - 1)
                if it_ >= 2:
                    attention(it_ - 2)
                if 1 <= it_ <= NT:
                    mask_transposes(it_ - 1)
        if stop_after == "C1":
            p.dma("sp", yTd.v, yT.v.rearrange("p c t -> p (c t)"))
            p.barrier()
            return nc, dbg

        with p.scope() as sg_:
            S_f = p.sb(sg_, "S_f", [128, 4, 256], F32)
            S_b = p.sb(sg_, "S_b", [128, 4, 256], BF)
            wgk = p.sb(sg_, "wgk", [16, 512], BF)
            bgk = p.sb(sg_, "bgk", [1, 512], BF)
            ones1 = p.sb(sg_, "ones1", [1, 128], BF)
            gng4 = p.sb(sg_, "gng4", [128, 4, 256], F32)
            p.memset("dve", S_f.v, 0.0)
            p.memset("dve", S_b.v, 0.0)
            p.memset("dve", ones1.v, 1.0)
            p.dma("pool", wgk.v, w_gk2.v)
            p.dma("pool", bgk.v, b_gk.v)
            for hh in range(4):
                p.dma("sp", gng4[:, hh, :], V(gla_g, gla_g.h.ap().partition_broadcast(128)))
            grs = [p.sb(sg_, "grs%d" % i, [16, 128], BF) for i in range(2)]
            gqs = [p.sb(sg_, "gqs%d" % i, [128, 4, 128], BF) for i in range(2)]
            gks = [p.sb(sg_, "gks%d" % i, [128, 4, 128], BF) for i in range(2)]
            vts = [p.sb(sg_, "vts%d" % i, [128, 1024], BF) for i in range(2)]
            gos = [p.sb(sg_, "gos%d" % i, [128, 1024], BF) for i in range(2)]
            lpe = p.sb(sg_, "lpe", [128, 512], F32)
            lp = p.sb(sg_, "lp", [128, 512], F32)
            E1 = p.sb(sg_, "E1", [128, 4, 128], F32)
            E2 = p.sb(sg_, "E2", [128, 4, 128], F32)
            qtl = p.sb(sg_, "qtl", [128, 4, 128], BF)
            ktl = p.sb(sg_, "ktl", [128, 4, 128], BF)
            ktok = p.sb(sg_, "ktok", [128, 4, 128], BF)
            ATs = p.sb(sg_, "ATs", [128, 4, 128], BF)
            tmpS = p.sb(sg_, "tmpS", [128, 256], F32)
            ssq = p.sb(sg_, "ssq", [128, 8], F32)
            gsil = p.sb(sg_, "gsil", [128, 4, 256], F32)
            ybt = p.sb(sg_, "ybt", [128, 1024], BF)
            junkg = p.sb(sg_, "junkg", [128, 256], BF)
            gqT_r = FT["gqT"].h.ap().rearrange("(c p) t -> p c t", p=128)
            gkT_r = FT["gkT"].h.ap().rearrange("(c p) t -> p c t", p=128)
            for ti in range(NT + 1):
                n = NM if ti == 0 else 128
                c0 = 0 if ti == 0 else NM + (ti - 1) * 128
                real = ti > 0
                gr = grs[ti % 2]; gq = gqs[ti % 2]; gk_ = gks[ti % 2]; vt = vts[ti % 2]; go = gos[ti % 2]
                p.dma("sp", gr[:, :n], FT["grT"][:, c0:c0 + n])
                p.dma("sp", gk_[:, :, :n], V(FT["gkT"], gkT_r[:, :, c0:c0 + n]))
                p.dma("sp", vt[:n, :], TM["gv"][c0:c0 + n, :])
                if real:
                    p.dma("sp", gq[:, :, :n], V(FT["gqT"], gqT_r[:, :, c0:c0 + n]))
                    p.dma("sp", go[:n, :], TM["go"][c0:c0 + n, :])
                bA = bank6()
                p.mm(bA[:n, :], gr[:, :n], wgk.v, start=True, stop=False)
                p.mm(bA[:n, :], ones1[:, :n], bgk.v, start=False, stop=True)
                p.act(lpe[:n, :], bA[:n, :], AF.Exp, scale=-1.0)
                p.act(lp[:n, :], lpe[:n, :], AF.Ln, bias=1.0)
                bB_ = bank6()
                for hh in range(4):
                    p.mm(bB_[:, hh * 128: hh * 128 + n], lp[:n, hh * 128:(hh + 1) * 128], tri_f[:n, :n])
                bBv = bB_.v.rearrange("p (h i) -> p h i", i=128)[:, :, :n]
                p.act(E1[:, :, :n], bBv, AF.Exp, scale=-1.0 / 16.0)
                p.act(E2[:, :, :n], bBv, AF.Exp, scale=1.0 / 16.0)
                p.tt("dve", ktl[:, :, :n], gk_[:, :, :n], E2[:, :, :n], ALU.mult)
                bT_ = bank6(); bTv = bT_.v.bitcast(BF)
                for hh in range(4):
                    p.tr(bTv[:n, hh * 128:(hh + 1) * 128], ktl[:, hh, :n], ident_b.v)
                p.copy("act", ktok[:n, :, :].rearrange("p h d -> p (h d)"), bTv[:n, 0:512])
                if real:
                    p.stt("dve", qtl[:, :, :n], gq[:, :, :n], 128.0 ** -0.5, E1[:, :, :n], ALU.mult, ALU.mult)
                    bC = bank6()
                    for hh in range(4):
                        p.mm(bC[:n, hh * 128: hh * 128 + n], ktl[:, hh, :n], qtl[:, hh, :n])
                    for hh in range(4):
                        p.tt("dve", ATs[:n, hh, :n], bC[:n, hh * 128: hh * 128 + n], tri_b[:n, :n], ALU.mult)
                    bO = [bank6(), bank6()]
                    for hh in range(4):
                        o_ = bO[hh // 2][:, (hh % 2) * 256:(hh % 2 + 1) * 256]
                        p.mm(o_, qtl[:, hh, :n], S_b[:, hh, :], start=True, stop=False)
                        p.mm(o_, ATs[:n, hh, :n], vt[:n, hh * 256:(hh + 1) * 256], start=False, stop=True)
                for hh in range(4):
                    bS = bank6()
                    p.mm(bS[:, 0:256], ktok[:n, hh, :], vt[:n, hh * 256:(hh + 1) * 256])
                    p.ts("dve", tmpS.v, bS[:, 0:256], E1[:, hh, n - 1:n], None, ALU.mult)
                    p.stt("dve", S_f[:, hh, :], S_f[:, hh, :], E1[:, hh, n - 1:n], tmpS.v, ALU.mult, ALU.add)
                p.copy("act", S_b.v, S_f.v)
                if real:
                    p.memset("dve", ssq[:, 0:4], 0.0)
                    for hh in range(4):
                        o_ = bO[hh // 2][:, (hh % 2) * 256:(hh % 2 + 1) * 256]
                        p.act(junkg.v, o_, AF.Square, accum=ssq[:, hh:hh + 1])
                    p.ts("dve", ssq[:, 4:8], ssq[:, 0:4], 1.0 / 256.0, EPS, ALU.mult, ALU.add)
                    p.act(ssq[:, 4:8], ssq[:, 4:8], AF.Sqrt)
                    p.recip(ssq[:, 4:8], ssq[:, 4:8])
                    p.act(gsil.v.rearrange("p h v -> p (h v)"), go.v, AF.Silu)
                    p.tt("dve", gsil.v, gsil.v, gng4.v, ALU.mult)
                    for hh in range(4):
                        o_ = bO[hh // 2][:, (hh % 2) * 256:(hh % 2 + 1) * 256]
                        p.stt("dve", ybt[:, hh * 256:(hh + 1) * 256], o_, ssq[:, 4 + hh:5 + hh], gsil[:, hh, :],
                              ALU.mult, ALU.mult)
                    bY = bank6(); bYv = bY.v.bitcast(BF)
                    for c in range(8):
                        p.tr(bYv[:, c * 128:(c + 1) * 128], ybt[:, c * 128:(c + 1) * 128], ident_b.v)
                    p.copy("act", yT[:, 8:16, (ti - 1) * 128: ti * 128], bYv.rearrange("p (c t) -> p c t", t=128))

        if stop_after == "C2":
            p.dma("sp", yTd.v, yT.v.rearrange("p c t -> p (c t)"))
            p.barrier()
            return nc, dbg

        with p.scope() as sd:
            wout = p.sb(sd, "wout", [128, 16, D], BF)
            w_out_r = w_out.h.ap().rearrange("(c p) n -> p c n", p=128)
            for j in range(4):
                p.dma("pool", wout[:, :, j * 512:(j + 1) * 512], V(w_out, w_out_r[:, :, j * 512:(j + 1) * 512]))
            gB1 = p.sb(sd, "gB1", [128, D], F32)
            bB1 = p.sb(sd, "bB1", [128, D], F32)
            p.dma("sp", gB1.v, V(ln1_g, ln1_g.h.ap().partition_broadcast(128)))
            p.dma("sp", bB1.v, V(ln1_b, ln1_b.h.ap().partition_broadcast(128)))
            hts = [p.sb(sd, "hts%d" % i, [128, D], F32) for i in range(2)]
            zb1 = p.sb(sd, "zb1", [128, D], BF)
            junkd = p.sb(sd, "junkd", [128, D], BF)
            std = p.sb(sd, "std", [128, 8], F32)
            junkd2 = p.sb(sd, "junkd2", [128, D], BF)
            std2 = p.sb(sd, "std2", [128, 8], F32)

            def d_mm(t):
                p.dma("sp", hts[t % 2].v, hd[t * 128:(t + 1) * 128, :])
                for j in range(4):
                    bk = banks[(t % 2) * 4 + j]
                    for c in range(16):
                        p.mm(bk.v, yT[:, c, t * 128:(t + 1) * 128], wout[:, c, j * 512:(j + 1) * 512],
                             start=(c == 0), stop=(c == 15))

            def d_res(t):
                ht = hts[t % 2]
                for j in range(4):
                    p.stt("dve", ht[:, j * 512:(j + 1) * 512], ht[:, j * 512:(j + 1) * 512], ALPHA,
                          banks[(t % 2) * 4 + j].v, ALU.mult, ALU.add)

            def d_ln(t):
                ht = hts[t % 2]
                layer_norm(128, ht.v, zb1.v, gB1.v, bB1.v, std if t % 2 else std2, junkd.v if t % 2 else junkd2.v)
                p.dma("sp", h1d[t * 128:(t + 1) * 128, :], ht.v)
                for half in range(2):
                    bk = banks[(t % 2) * 4 + half]; bv = bk.v.bitcast(BF)
                    for c in range(8):
                        cc = half * 8 + c
                        p.tr(bv[:, c * 128:(c + 1) * 128], zb1[:, cc * 128:(cc + 1) * 128], ident_b.v)
                    p.copy("act", yT[:, half * 8:(half + 1) * 8, t * 128:(t + 1) * 128],
                           bv.rearrange("p (c t) -> p c t", t=128))

            d_mm(0)
            d_res(0)
            for t in range(NT):
                if t + 1 < NT:
                    d_mm(t + 1)
                d_ln(t)
                if t + 1 < NT:
                    d_res(t + 1)
        if stop_after == "D":
            p.barrier()
            return nc, dbg

        h1T = yT
        sD = scratch("sD", [S, 2048], F32)
        with p.scope() as s1_:
            qhT = p.sb(s1_, "qhT", [128, 16, S], BF)
            skn = p.sb(s1_, "skn", [128, 16, 128], BF)
            skT = p.sb(s1_, "skT", [128, 16, 128], BF)
            p.dma("pool", skn.v, V(sub_keys, sub_keys.h.ap().rearrange("a n c -> n a c")))
            for g in range(2):
                bk = bank(); bv = bk.v.bitcast(BF)
                for j in range(8):
                    p.tr(bv[:, j * 128:(j + 1) * 128], skn[:, g * 8 + j, :], ident_b.v)
                p.copy("act", skT[:, g * 8:(g + 1) * 8, :].rearrange("p a n -> p (a n)"), bv)
            wps = [p.sb(s1_, "wps%d" % i, [128, 16, 128], BF) for i in range(3)]
            w_pq_r = w_pq.h.ap().rearrange("(c p) n -> p c n", p=128)
            for hs in range(16):
                w = wps[hs % 3]
                p.dma("pool", w.v, V(w_pq, w_pq_r[:, :, hs * 128:(hs + 1) * 128]))
                for tb in range(4):
                    bk = bank()
                    for c in range(16):
                        p.mm(bk.v, w[:, c, :], h1T[:, c, tb * 512:(tb + 1) * 512], start=(c == 0), stop=(c == 15))
                    if (hs + tb) % 2:
                        p.copy("act", qhT[:, hs, tb * 512:(tb + 1) * 512], bk.v)
                    else:
                        p.copy("dve", qhT[:, hs, tb * 512:(tb + 1) * 512], bk.v)
            sst = [p.sb(s1_, "sst%d" % i, [128, 2048], F32) for i in range(2)]
            for t in range(NT):
                ss_ = sst[t % 2]
                for g in range(4):
                    bk = bank()
                    for j in range(4):
                        hs = g * 4 + j
                        p.mm(bk[:, j * 128:(j + 1) * 128], qhT[:, hs, t * 128:(t + 1) * 128], skT[:, hs, :])
                    if g % 2:
                        p.copy("act", ss_[:, g * 512:(g + 1) * 512], bk.v)
                    else:
                        p.copy("dve", ss_[:, g * 512:(g + 1) * 512], bk.v)
                p.dma("sp", sD[t * 128:(t + 1) * 128, :], ss_.v)
        if stop_after == "P1":
            p.barrier()
            return nc, dbg
        h1Td = scratch("h1Td", [128, 16 * S], BF)
        p.dma("sp", h1Td.v, yT.v.rearrange("p c t -> p (c t)"))
        p.barrier()
        p.release(yT)
        sY.close()
        GT3 = scratch("GT3", [NT, 128, 128 * 128], BF)
        with p.scope() as s2_:
            sts = [p.sb(s2_, "sts%d" % i, [128, 16, 128], F32) for i in range(2)]
            wa = p.sb(s2_, "wa", [128, 128], F32)
            t16 = p.sb(s2_, "t16", [128, 8, 2, 16], F32)
            cand = p.sb(s2_, "cand", [128, 256], F32)
            cw = p.sb(s2_, "cw", [128, 256], F32)
            c16 = p.sb(s2_, "c16", [128, 8, 16], F32)
            exs = p.sb(s2_, "exs", [128, 8, 16], F32)
            sm = p.sb(s2_, "sm", [128, 4, 8], F32)
            theta = p.sb(s2_, "theta", [128, 8, 16], F32)
            E_ = p.sb(s2_, "E_", [128, 16, 128], F32)
            OAfs = [p.sb(s2_, "OAf%d" % i, [128, 128, 64], BF) for i in range(2)]
            OBfs = [p.sb(s2_, "OBf%d" % i, [128, 128, 64], BF) for i in range(2)]
            OAp = p.sb(s2_, "OAp", [128, 128, 128], BF)
            OBp = p.sb(s2_, "OBp", [128, 128, 128], BF)
            Gs = p.sb(s2_, "Gs", [128, 128, 128], BF)
            ev = [0]
            for t in range(NT):
                st_ = sts[t % 2]
                if t == 0:
                    p.dma("sp", st_.v.rearrange("p a n -> p (a n)"), sD[0:128, :])
                if t + 1 < NT:
                    p.dma("sp", sts[(t + 1) % 2].v.rearrange("p a n -> p (a n)"), sD[(t + 1) * 128:(t + 2) * 128, :])
                st4 = st_.v.rearrange("p (h s) n -> p h s n", s=2)
                for hh in range(8):
                    for sd_ in range(2):
                        sv = st_[:, hh * 2 + sd_, :]
                        p.max8(t16[:, hh, sd_, 0:8], sv)
                        p.mrep(wa.v, t16[:, hh, sd_, 0:8], sv, -3.0e38)
                        p.max8(t16[:, hh, sd_, 8:16], wa.v)
                    cv = cand.v.rearrange("p (a b) -> p a b", b=16)
                    in0 = V(t16, t16.h[:, hh, 0, :].unsqueeze(2).to_broadcast([128, 16, 16]))
                    in1 = V(t16, t16.h[:, hh, 1, :].unsqueeze(1).to_broadcast([128, 16, 16]))
                    p.tt("dve", cv, in0, in1, ALU.add)
                    p.max8(c16[:, hh, 0:8], cand.v)
                    p.mrep(cw.v, c16[:, hh, 0:8], cand.v, -3.0e38)
                    p.max8(c16[:, hh, 8:16], cw.v)
                p.tt("dve", exs.v, c16.v, V(c16, c16.h[:, :, 0:1].to_broadcast([128, 8, 16])), ALU.subtract)
                p.act(exs.v, exs.v, AF.Exp)
                p.rsum("dve", sm[:, 0, :], exs.v)
                p.recip(sm[:, 1, :], sm[:, 0, :])
                p.ts("dve", sm[:, 2, :], c16[:, :, 15], -2.0e-5, None, ALU.add)
                p.tt("dve", theta.v, V(sm, sm.h[:, 2, :].unsqueeze(2).to_broadcast([128, 8, 16])), t16[:, :, 0, :],
                     ALU.subtract)
                m16 = t16.h[:, :, :, 0:1].rearrange("p h s o -> p (h s) o").to_broadcast([128, 16, 128])
                p.tt("dve", E_.v, st_.v, V(t16, m16), ALU.subtract)
                p.act(E_.v, E_.v, AF.Exp)
                E4 = E_.v.rearrange("p (h s) n -> p h s n", s=2)
                p.tt("dve", E4[:, :, 0, :], E4[:, :, 0, :],
                     V(sm, sm.h[:, 1, :].unsqueeze(2).to_broadcast([128, 8, 128])), ALU.mult)
                for hf_ in range(2):
                    OAf = OAfs[hf_]; OBf = OBfs[hf_]
                    isl = slice(hf_ * 64, (hf_ + 1) * 64)
                    for qd in range(4):
                        hs_ = slice(qd * 2, qd * 2 + 2)
                        OA = V(OAf, OAf.h[:, qd * 32:(qd + 1) * 32, :].rearrange("t (h a) i -> t h a i", h=2))
                        OB = V(OBf, OBf.h[:, qd * 32:(qd + 1) * 32, :].rearrange("t (h a) i -> t h a i", h=2))
                        sa_b = V(st_, st4.ap[:, hs_, 0, isl].unsqueeze(2).to_broadcast([128, 2, 16, 64]))
                        sb_b = V(st_, st4.ap[:, hs_, 1, isl].unsqueeze(2).to_broadcast([128, 2, 16, 64]))
                        s1_b = V(t16, t16.h[:, hs_, 0, :].unsqueeze(3).to_broadcast([128, 2, 16, 64]))
                        th_b = V(theta, theta.h[:, hs_, :].unsqueeze(3).to_broadcast([128, 2, 16, 64]))
                        ea_b = V(E_, E4.ap[:, hs_, 0, isl].unsqueeze(2).to_broadcast([128, 2, 16, 64]))
                        eb_b = V(E_, E4.ap[:, hs_, 1, isl].unsqueeze(2).to_broadcast([128, 2, 16, 64]))
                        p.tt("dve", OA, sa_b, s1_b, ALU.is_equal)
                        p.tt("dve", OA, OA, ea_b, ALU.mult)
                        p.tt("dve", OB, sb_b, th_b, ALU.is_ge)
                        p.tt("dve", OB, OB, eb_b, ALU.mult)
                    for (src, dstp) in ((OAf, OAp), (OBf, OBp)):
                        for ig in range(8):
                            bk = bank(); bv = bk.v.bitcast(BF)
                            for j in range(8):
                                p.tr(bv[:, j * 128:(j + 1) * 128], src[:, :, ig * 8 + j], ident_b.v)
                            i0_ = hf_ * 64 + ig * 8
                            p.copy("act", dstp[:, i0_:i0_ + 8, :].rearrange("p i t -> p (i t)"), bv)
                for tl in range(128):
                    if tl % 4 == 0:
                        bk = bank()
                        bkv = bk.v.rearrange("p (j f) -> p j f", f=4)
                    p.mm(bkv[:, :, tl % 4], OAp[:, :, tl], OBp[:, :, tl])
                    if tl % 4 == 3:
                        ev[0] += 1
                        p.copy("act", Gs[:, :, tl - 3:tl + 1], bkv)
                p.dma("act", GT3[t], Gs.v.rearrange("p j t -> p (j t)"))
        if stop_after == "P2":
            p.barrier()
            return nc, dbg
        IG = 4
        u_r = u_tab.h.ap().rearrange("(i j) d -> j i d", j=128)
        v_r = v_tab.h.ap().rearrange("(i j) d -> j i d", j=128)
        sH = contextlib.ExitStack()
        sH.tiles = []
        es.enter_context(sH)
        h1T = p.sb(sH, "h1T", [128, 16, S], BF)
        p.dma("sp", h1T.v.rearrange("p c t -> p (c t)"), h1Td.v)
        for hf in range(2):
            with p.scope() as s3_:
                acc = p.sb(s3_, "acc", [128, 8, D], F32)
                with p.scope() as s4_:
                    urows = [p.sb(s4_, "urow%d" % i, [128, D], BF) for i in range(3)]
                    uTs = [p.sb(s4_, "uT%d" % i, [128, 16, 128], BF) for i in range(2)]
                    gti = [p.sb(s4_, "gti%d" % i, [128, 1024], BF) for i in range(2)]
                    gel = [p.sb(s4_, "gel%d" % i, [128, 512], BF) for i in range(2)]
                    GHs = [p.sb(s4_, "GH%d" % i, [128, IG, 1024], BF) for i in range(2)]
                    vss = [p.sb(s4_, "vs%d" % i, [128, IG, D], BF) for i in range(2)]
                    gi = [0]

                    def load(i_):
                        ur = urows[i_ % 3]
                        vs = vss[(i_ // IG) % 2]
                        p.dma("pool", ur.v, V(u_tab, u_r[i_]))
                        p.dma("pool", vs[:, i_ % IG, :], V(v_tab, v_r[i_]))

                    def prep(i_):
                        ur = urows[i_ % 3]; uT = uTs[i_ % 2]; gt = gti[i_ % 2]
                        src_g = GT3.h[hf * 8:(hf + 1) * 8, :, i_ * 128:(i_ + 1) * 128].rearrange("a i t -> i a t")
                        p.dma("sp", gt.v.rearrange("p (a t) -> p a t", t=128), V(GT3, src_g))
                        for g in range(2):
                            bk = bank(); bv = bk.v.bitcast(BF)
                            for j in range(8):
                                c = g * 8 + j
                                p.tr(bv[:, j * 128:(j + 1) * 128], ur[:, c * 128:(c + 1) * 128], ident_b.v)
                            p.copy("act", uT[:, g * 8:(g + 1) * 8, :].rearrange("p c j -> p (c j)"), bv)

                    def hidden(i_):
                        uT = uTs[i_ % 2]; gt = gti[i_ % 2]
                        GH = GHs[(i_ // IG) % 2]; il = i_ % IG
                        for tb in range(2):
                            t0 = hf * 1024 + tb * 512
                            bk = bank()
                            for c in range(16):
                                p.mm(bk.v, uT[:, c, :], h1T[:, c, t0:t0 + 512], start=(c == 0), stop=(c == 15))
                            ge = gel[gi[0] % 2]; gi[0] += 1
                            p.act(ge.v, bk.v, AF.Gelu)
                            p.tt("dve", GH[:, il, tb * 512:(tb + 1) * 512], ge.v, gt[:, tb * 512:(tb + 1) * 512],
                                 ALU.mult)

                    def outmm(ig, tt_):
                        GH = GHs[ig % 2]; vs = vss[ig % 2]
                        bks = [bank() for _ in range(4)]
                        for j in range(4):
                            for il in range(IG):
                                p.mm(bks[j].v, GH[:, il, tt_ * 128:(tt_ + 1) * 128], vs[:, il, j * 512:(j + 1) * 512],
                                     start=(il == 0), stop=(il == IG - 1))
                        for j in range(4):
                            a_ = acc[:, tt_, j * 512:(j + 1) * 512]
                            if ig == 0:
                                p.copy("dve", a_, bks[j].v)
                            else:
                                p.tt("dve", a_, a_, bks[j].v, ALU.add)

                    sched = {0: [0, 1, 2], 1: [3, 4, 5], 2: [6, 7], 3: []}
                    load(0)
                    load(1)
                    prep(0)
                    for i_ in range(128):
                        ig = i_ // IG
                        if i_ + 1 < 128:
                            prep(i_ + 1)
                        hidden(i_)
                        if ig >= 1:
                            for k in sched[i_ % IG]:
                                outmm(ig - 1, k)
                        if i_ + 2 < 128:
                            load(i_ + 2)
                    for tt_ in range(8):
                        outmm(128 // IG - 1, tt_)
                with p.scope() as s5_:
                    gB2 = p.sb(s5_, "gB2", [128, D], F32)
                    bB2 = p.sb(s5_, "bB2", [128, D], F32)
                    p.dma("sp", gB2.v, V(ln2_g, ln2_g.h.ap().partition_broadcast(128)))
                    p.dma("sp", bB2.v, V(ln2_b, ln2_b.h.ap().partition_broadcast(128)))
                    h1s = [p.sb(s5_, "h1s%d" % i, [128, D], F32) for i in range(2)]
                    ots = [p.sb(s5_, "ots%d" % i, [128, D], F32) for i in range(2)]
                    junk2s = [p.sb(s5_, "junk2%d" % i, [128, D], BF) for i in range(2)]
                    st2s = [p.sb(s5_, "st2%d" % i, [128, 8], F32) for i in range(2)]
                    for tt_ in range(8):
                        t = hf * 8 + tt_
                        h1 = h1s[tt_ % 2]; ot = ots[tt_ % 2]
                        p.dma("sp", h1.v, h1d[t * 128:(t + 1) * 128, :])
                        p.stt("dve", h1.v, h1.v, ALPHA, acc[:, tt_, :], ALU.mult, ALU.add)
                        layer_norm(128, h1.v, ot.v, gB2.v, bB2.v, st2s[tt_ % 2], junk2s[tt_ % 2].v)
                        p.dma("sp", out_d[t * 128:(t + 1) * 128, :], ot.v)


        p.barrier()
    return nc, dbg


def make_consts():
    ident = np.eye(128, dtype=np.float32)
    tri = np.triu(np.ones((128, 128), dtype=np.float32))
    boh = np.zeros((32, 512), dtype=np.float32)
    rlt = [[15, 165], [14, 27], [13, 18], [12, 14], [11, 9], [10, 7], [9, 4], [8, 4], [7, 1], [6, 1], [5, 1],
           [4, 1], [3, 1], [2, 1], [1, 1], [0, 1], [17, 1], [18, 1], [19, 1], [20, 1], [21, 1], [22, 1], [23, 1],
           [24, 4], [25, 4], [26, 7], [27, 9], [28, 14], [29, 18], [30, 27], [31, 37]]
    bucket = []
    for v, n in rlt:
        bucket += [v] * n
    for i in range(383):
        boh[bucket[i], i] = 1.0
    jrev = np.ascontiguousarray(np.eye(128, dtype=np.float32)[::-1])
    return {"c_ident": ident, "c_tri": tri, "c_boh": boh, "c_jrev": jrev}


def core_inputs(inputs, b):
    m = {
        "x": np.ascontiguousarray(inputs["x"][b]),
        "meta_tokens": np.ascontiguousarray(inputs["meta_tokens"]),
        "ln0_g": inputs["ln0_g"].reshape(1, D), "ln0_b": inputs["ln0_b"].reshape(1, D),
        "rel_bias": np.ascontiguousarray(inputs["rel_bias"]),
        "w_in": np.ascontiguousarray(inputs["w_in"][0]),
        "w_uk": np.ascontiguousarray(inputs["w_uk"][0]), "w_uv": np.ascontiguousarray(inputs["w_uv"][0]),
        "w_gk2": np.ascontiguousarray(inputs["w_gk2"][0]), "b_gk": inputs["b_gk"].reshape(1, 512),
        "gla_norm_g": inputs["gla_norm_g"].reshape(1, 256),
        "w_out": np.ascontiguousarray(inputs["w_out"][0]),
        "ln1_g": inputs["ln1_g"].reshape(1, D), "ln1_b": inputs["ln1_b"].reshape(1, D),
        "w_pq": np.ascontiguousarray(inputs["w_pq"][0]),
        "sub_keys": np.ascontiguousarray(inputs["sub_keys"][0]).reshape(16, 128, 128),
        "u_tab": np.ascontiguousarray(inputs["u_tab"][0]), "v_tab": np.ascontiguousarray(inputs["v_tab"][0]),
        "ln2_g": inputs["ln2_g"].reshape(1, D), "ln2_b": inputs["ln2_b"].reshape(1, D),
    }
    m.update(make_consts())
    return {k: np.asarray(v, dtype=np.float32) for k, v in m.items()}


def kernel(**inputs):
    inputs = {k: np.asarray(v) for k, v in inputs.items()}
    nc, _ = build()
    in_maps = [core_inputs(inputs, b) for b in range(8)]
    res = run_bass_kernel_spmd(nc, in_maps, core_ids=list(range(8)))
    return np.stack([np.asarray(r["out"]) for r in res.results], axis=0).astype(np.float32)
```

```python
import contextlib
import math
import numpy as np
import ml_dtypes
import concourse.bass as bass
import concourse.mybir as mybir
from concourse.bass_utils import run_bass_kernel_spmd

F32 = mybir.dt.float32
BF = mybir.dt.bfloat16
ALU = mybir.AluOpType
AF = mybir.ActivationFunctionType
AX = mybir.AxisListType

D = 2048
S = 2048
NM = 16
L = S + NM
NT = S // 128
EPS = 1e-5
ALPHA = 2.0 ** 0.25
IN_COLS = 5472
O_AQ, O_CKV, O_IQ, O_IK, O_IW, O_GQ, O_GK, O_GV, O_GR, O_GO = (
    0, 1024, 1280, 2304, 2368, 2384, 2896, 3408, 4432, 4448)
NEG = -1.0e30


class Trk:
    def __init__(self, h, name, sb):
        self.h = h
        self.name = name
        self.sb = sb
        self.w = {}
        self.r = {}
        self.dsem = None

    def __getitem__(self, k):
        return V(self, self.h[k])

    @property
    def v(self):
        return V(self, self.h.ap() if hasattr(self.h, "ap") else self.h[:])


class V:
    def __init__(self, o, ap):
        self.o = o
        self.ap = ap

    def __getitem__(self, k):
        return V(self.o, self.ap[k])

    def bitcast(self, dt):
        return V(self.o, self.ap.bitcast(dt))

    def rearrange(self, pattern_, **kw):
        return V(self.o, self.ap.rearrange(pattern_, **kw))

    def bc(self, shape):
        return V(self.o, self.ap.to_broadcast(shape))


class Prog:
    def __init__(self, nc, es):
        self.nc = nc
        self.es = es
        self.e = dict(pe=nc.tensor, act=nc.scalar, dve=nc.vector, pool=nc.gpsimd, sp=nc.sync)
        self.sem = {}
        self.cnt = {}
        for k in ("pe", "act", "dve", "pool"):
            self.sem[k] = es.enter_context(nc.semaphore("S_" + k))
            self.cnt[k] = 0
        self.waited = {k: {} for k in self.e}
        self.dfree = []
        self.dfree_sw = []
        self.ndsem = 0
        self.bank_i = 0
        self.n_inst = 0

    def sb(self, stack, name, shape, dt):
        self.n_sb = getattr(self, "n_sb", 0) + 1
        name = "%s_%d" % (name, self.n_sb)
        h = stack.enter_context(self.nc.sbuf_tensor(name, list(shape), dt))
        t = Trk(h, name, True)
        if hasattr(stack, "tiles"):
            stack.tiles.append(t)
        return t

    def dram(self, name, shape, dt, kind=None):
        if kind:
            h = self.nc.dram_tensor(name, list(shape), dt, kind=kind)
        else:
            h = self.nc.dram_tensor(name, list(shape), dt)
        return Trk(h, name, False)

    def _get_dsem(self, t, sw):
        if t.dsem is None:
            t.dsem = {}
        if sw not in t.dsem:
            free = self.dfree_sw if sw else self.dfree
            if free:
                t.dsem[sw] = free.pop()
            else:
                self.ndsem += 1
                key = ("W%d" if sw else "D%d") % self.ndsem
                self.sem[key] = self.es.enter_context(self.nc.semaphore(key))
                self.cnt[key] = 0
                t.dsem[sw] = key
        return t.dsem[sw]

    @contextlib.contextmanager
    def scope(self):
        st = contextlib.ExitStack()
        st.tiles = []
        try:
            yield st
        finally:
            self.barrier()
            self.release(*st.tiles)
            st.close()

    def release(self, *ts):
        for t in ts:
            if t.dsem is not None:
                for sw, key in t.dsem.items():
                    (self.dfree_sw if sw else self.dfree).append(key)
                t.dsem = None

    def _wait(self, eng, key, val):
        if self.waited[eng].get(key, 0) >= val:
            return
        self.e[eng].wait_ge(self.sem[key], val)
        self.waited[eng][key] = val

    def _sync(self, eng, reads, writes):
        need = {}
        for t in reads:
            for k, v in t.w.items():
                if need.get(k, 0) < v:
                    need[k] = v
        for t in writes:
            for k, v in t.w.items():
                if need.get(k, 0) < v:
                    need[k] = v
            for k, v in t.r.items():
                if need.get(k, 0) < v:
                    need[k] = v
        for k, v in need.items():
            if k == eng and eng == "pe":
                continue
            self._wait(eng, k, v)

    def _done(self, key, val, reads, writes):
        for t in reads:
            t.r[key] = val
        for t in writes:
            t.w = {key: val}
            t.r = {}

    def op(self, eng, reads, writes, fn):
        reads = [x.o for x in reads if isinstance(x, V)]
        writes = [x.o for x in writes]
        self._sync(eng, reads, writes)
        ins = fn(self.e[eng])
        self.cnt[eng] += 1
        ins.then_inc(self.sem[eng], 1)
        self._done(eng, self.cnt[eng], reads, writes)
        self.n_inst += 1

    def dma(self, q, out, in_, **kw):
        sbt = out.o if out.o.sb else in_.o
        self._sync(q, [in_.o], [out.o])
        ins = self.e[q].dma_start(out=out.ap, in_=in_.ap, **kw)
        key = self._get_dsem(sbt, q == "pool")
        self.cnt[key] += 16
        ins.then_inc(self.sem[key], 16)
        self._done(key, self.cnt[key], [in_.o], [out.o])
        self.n_inst += 1

    def barrier(self, engines=("pe", "act", "dve", "pool", "sp")):
        for e in engines:
            for k, v in self.cnt.items():
                if v > 0 and k != e:
                    self._wait(e, k, v)

    def mm(self, out, lhsT, rhs, start=True, stop=True):
        self.op("pe", [lhsT, rhs], [out],
                lambda e: e.matmul(out.ap, lhsT.ap, rhs.ap, start=start, stop=stop))

    def tr(self, out, in_, ident):
        self.op("pe", [in_, ident], [out], lambda e: e.transpose(out.ap, in_.ap, ident.ap))

    def act(self, out, in_, func, bias=0.0, scale=1.0, accum=None, eng="act"):
        rd = [in_, bias, scale]
        wr = [out] + ([accum] if accum is not None else [])
        b = bias.ap if isinstance(bias, V) else bias
        sc = scale.ap if isinstance(scale, V) else scale
        kw = {}
        if accum is not None:
            kw["accum_out"] = accum.ap
        self.op("act", rd, wr, lambda e: e.activation(out.ap, in_.ap, func, bias=b, scale=sc, **kw))

    def tt(self, eng, out, in0, in1, op):
        self.op(eng, [in0, in1], [out], lambda e: e.tensor_tensor(out.ap, in0.ap, in1.ap, op))

    def ts(self, eng, out, in0, s1, s2, op0, op1=None, accum=None):
        a1 = s1.ap if isinstance(s1, V) else s1
        a2 = s2.ap if isinstance(s2, V) else s2
        kw = {}
        if op1 is not None:
            kw["op1"] = op1
        if accum is not None:
            kw["accum_out"] = accum.ap
        wr = [out] + ([accum] if accum is not None else [])
        self.op(eng, [in0, s1, s2], wr,
                lambda e: e.tensor_scalar(out.ap, in0.ap, a1, a2, op0, **kw))

    def stt(self, eng, out, in0, sc, in1, op0, op1):
        a = sc.ap if isinstance(sc, V) else sc
        self.op(eng, [in0, sc, in1], [out],
                lambda e: e.scalar_tensor_tensor(out.ap, in0.ap, a, in1.ap, op0, op1))

    def copy(self, eng, out, in_):
        if eng == "act":
            self.op(eng, [in_], [out], lambda e: e.copy(out.ap, in_.ap))
        else:
            self.op(eng, [in_], [out], lambda e: e.tensor_copy(out.ap, in_.ap))

    def memset(self, eng, out, val):
        self.op(eng, [], [out], lambda e: e.memset(out.ap, val))

    def rsum(self, eng, out, in_):
        self.op(eng, [in_], [out], lambda e: e.reduce_sum(out.ap, in_.ap, AX.X))

    def recip(self, out, in_):
        self.op("dve", [in_], [out], lambda e: e.reciprocal(out.ap, in_.ap))

    def max8(self, out, in_):
        self.op("dve", [in_], [out], lambda e: e.max(out=out.ap, in_=in_.ap))

    def mrep(self, out, rep, vals, imm):
        self.op("dve", [rep, vals], [out],
                lambda e: e.match_replace(out=out.ap, in_to_replace=rep.ap, in_values=vals.ap,
                                          imm_value=imm))


def build(debug=None, stop_after=None):
    nc = bass.Bass("TRN2", target_bir_lowering=False)
    es = contextlib.ExitStack()
    with es:
        p = Prog(nc, es)
        IN = {}

        def inp(name, shape, dt=F32):
            IN[name] = p.dram(name, shape, dt, kind="ExternalInput")
            return IN[name]

        x = inp("x", [S, D])
        meta = inp("meta_tokens", [NM, D])
        ln0_g = inp("ln0_g", [1, D]); ln0_b = inp("ln0_b", [1, D])
        rel_bias = inp("rel_bias", [32, 8])
        w_in = inp("w_in", [D, IN_COLS])
        w_uk = inp("w_uk", [8, 256, 128]); w_uv = inp("w_uv", [8, 256, 128])
        w_gk2 = inp("w_gk2", [16, 512]); b_gk = inp("b_gk", [1, 512])
        gla_g = inp("gla_norm_g", [1, 256])
        w_out = inp("w_out", [D, D])
        ln1_g = inp("ln1_g", [1, D]); ln1_b = inp("ln1_b", [1, D])
        w_pq = inp("w_pq", [D, D])
        sub_keys = inp("sub_keys", [16, 128, 128])
        u_tab = inp("u_tab", [16384, D]); v_tab = inp("v_tab", [16384, D])
        ln2_g = inp("ln2_g", [1, D]); ln2_b = inp("ln2_b", [1, D])
        ident_in = inp("c_ident", [128, 128])
        tri_in = inp("c_tri", [128, 128])
        boh_in = inp("c_boh", [32, 512])
        jrev_in = inp("c_jrev", [128, 128])
        out_d = p.dram("out", [S, D], F32, kind="ExternalOutput")

        dbg = {}

        def scratch(name, shape, dt):
            kind = "ExternalOutput" if (debug and name in debug) else None
            t = p.dram(name, shape, dt, kind=kind)
            dbg[name] = t
            return t

        blk = es.enter_context(nc.Block())
        gs = contextlib.ExitStack()
        es.enter_context(gs)
        banks = []
        for i in range(8):
            h = es.enter_context(nc.psum_tensor("bank%d" % i, [128, 512], F32))
            banks.append(Trk(h, "bank%d" % i, True))

        def bank():
            b = banks[p.bank_i % 8]
            p.bank_i += 1
            return b

        ident_f = p.sb(gs, "ident_f", [128, 128], F32)
        ident_b = p.sb(gs, "ident_b", [128, 128], BF)
        tri_f = p.sb(gs, "tri_f", [128, 128], F32)
        tri_b = p.sb(gs, "tri_b", [128, 128], BF)
        p.dma("sp", ident_f.v, ident_in.v)
        p.dma("sp", tri_f.v, tri_in.v)
        p.copy("dve", ident_b.v, ident_f.v)
        p.copy("dve", tri_b.v, tri_f.v)
        jrev_b = p.sb(gs, "jrev_b", [128, 128], BF)
        p.dma("pool", jrev_b.v, jrev_in.v)

        def layer_norm(n, src, dst, gB, bB, st, junk):
            p.rsum("dve", st[:n, 0:1], src)
            p.ts("dve", st[:n, 1:2], st[:n, 0:1], -1.0 / D, None, ALU.mult)
            p.act(src, src, AF.Identity, bias=st[:n, 1:2])
            p.memset("dve", st[:n, 2:3], 0.0)
            p.act(junk, src, AF.Square, accum=st[:n, 2:3])
            p.ts("dve", st[:n, 3:4], st[:n, 2:3], 1.0 / D, EPS, ALU.mult, ALU.add)
            p.act(st[:n, 5:6], st[:n, 3:4], AF.Sqrt)
            p.recip(st[:n, 4:5], st[:n, 5:6])
            p.stt("dve", src, src, st[:n, 4:5], gB, ALU.mult, ALU.mult)
            p.tt("dve", src, src, bB, ALU.add)
            p.copy("act", dst, src)

        FT = {}
        for nm, rows in (("qlT", 2048), ("ckvT", 256), ("iqT", 1024), ("ikT", 128),
                         ("gqT", 512), ("gkT", 512), ("grT", 16)):
            FT[nm] = scratch(nm, [rows, L], BF)
        TM = {}
        TM["ckv"] = scratch("ckv", [L, 256], BF)
        TM["iw"] = scratch("iw", [L, 16], F32)
        TM["gv"] = scratch("gv", [L, 1024], BF)
        TM["go"] = scratch("go", [L, 1024], BF)
        hd = scratch("hd", [S, D], F32)
        h1d = scratch("h1d", [S, D], F32)

        sA = contextlib.ExitStack()
        sA.tiles = []
        hTm = p.sb(sA, "hTm", [128, 16, NM], BF)
        hTb = [p.sb(sA, "hTb%d" % i, [128, 16, 512], BF) for i in range(4)]

        def hT_cols(c0, n):
            if c0 < NM:
                assert c0 + n <= NM
                return hTm, slice(c0, c0 + n)
            b = (c0 - NM) // 512
            o = (c0 - NM) % 512
            assert o + n <= 512
            return hTb[b], slice(o, o + n)
        if True:
            s0 = sA
            gB = p.sb(s0, "gB", [128, D], F32)
            bB = p.sb(s0, "bB", [128, D], F32)
            p.dma("sp", gB.v, V(ln0_g, ln0_g.h.ap().partition_broadcast(128)))
            p.dma("sp", bB.v, V(ln0_b, ln0_b.h.ap().partition_broadcast(128)))
            xts = [p.sb(s0, "xt%d" % i, [128, D], F32) for i in range(2)]
            zts = [p.sb(s0, "zt%d" % i, [128, D], BF) for i in range(2)]
            junks = [p.sb(s0, "junk%d" % i, [128, D], BF) for i in range(2)]
            sts0 = [p.sb(s0, "st%d" % i, [128, 8], F32) for i in range(2)]
            for ti in range(NT + 1):
                n = NM if ti == 0 else 128
                c0 = 0 if ti == 0 else NM + (ti - 1) * 128
                xt = xts[ti % 2]; zt = zts[ti % 2]
                if ti == 0:
                    p.dma("sp", xt[:n, :], meta.v)
                else:
                    p.dma("sp", xt[:n, :], x[(ti - 1) * 128: ti * 128, :])
                layer_norm(n, xt[:n, :], zt[:n, :], gB[:n, :], bB[:n, :], sts0[ti % 2], junks[ti % 2][:n, :])
                if ti > 0:
                    p.dma("sp", hd[(ti - 1) * 128: ti * 128, :], xt.v)
                for half in range(2):
                    bk = bank()
                    bv = bk.v.bitcast(BF)
                    for c in range(8):
                        cc = half * 8 + c
                        p.tr(bv[:, c * 128: c * 128 + n], zt[:n, cc * 128:(cc + 1) * 128],
                             ident_b[:n, :n])
                    src = bv.rearrange("p (c t) -> p c t", t=128)[:, :, :n]
                    ht_, sl_ = hT_cols(c0, n)
                    p.copy("act", ht_[:, half * 8:(half + 1) * 8, sl_], src)
        with p.scope() as s1:
            wsl = [p.sb(s1, "wsl%d" % i, [128, 16, 128], BF) for i in range(3)]
            stg = [p.sb(s1, "stg%d" % i, [128, 512], BF) for i in range(4)]
            stgf = [p.sb(s1, "stgf%d" % i, [128, 16], F32) for i in range(2)]
            wukT = p.sb(s1, "wukT", [128, 8, 256], BF)
            w_in_r = w_in.h.ap().rearrange("(c p) n -> p c n", p=128)
            with p.scope() as s2:
                wuk_n = p.sb(s2, "wuk_n", [128, 16, 128], BF)
                p.dma("pool", wuk_n.v, V(w_uk, w_uk.h.ap().rearrange("h (rc p) d -> p (h rc) d", p=128)))
                for g in range(2):
                    bk = bank(); bv = bk.v.bitcast(BF)
                    for j in range(8):
                        hr = g * 8 + j
                        p.tr(bv[:, j * 128:(j + 1) * 128], wuk_n[:, hr, :], ident_b.v)
                    p.copy("dve", wukT[:, g * 4:(g + 1) * 4, :].rearrange("p h r -> p (h r)"), bv)
            wi = [0]
            si = [0]

            def load_w(col0, ncols, dup=False):
                w = wsl[wi[0] % 3]; wi[0] += 1
                if dup:
                    p.dma("pool", w[:, :, 0:ncols], V(w_in, w_in_r[:, :, col0:col0 + ncols]))
                    p.dma("pool", w[:, :, ncols:2 * ncols], V(w_in, w_in_r[:, :, col0:col0 + ncols]))
                else:
                    p.dma("pool", w[:, :, 0:ncols], V(w_in, w_in_r[:, :, col0:col0 + ncols]))
                return w

            tok_blocks = [(NM + i * 512, 512) for i in range(4)] + [(0, NM)]

            def fm_group(col0, rows, dst, dst_row0, real_only=False, dup=False, post=None):
                w = load_w(col0, rows // 2 if dup else rows, dup)
                for (t0, tn) in tok_blocks:
                    if real_only and t0 == 0:
                        continue
                    bk = bank()
                    ht_, sl_ = hT_cols(t0, tn)
                    for c in range(16):
                        p.mm(bk[:rows, :tn], w[:, c, :rows], ht_[:, c, sl_],
                             start=(c == 0), stop=(c == 15))
                    sg = stg[si[0] % 4]; si[0] += 1
                    if si[0] % 2:
                        p.copy("act", sg[:rows, :tn], bk[:rows, :tn])
                    else:
                        p.copy("dve", sg[:rows, :tn], bk[:rows, :tn])
                    if post is not None:
                        post(sg, t0, tn)
                    else:
                        p.dma("sp", dst[dst_row0:dst_row0 + rows, t0:t0 + tn], sg[:rows, :tn])

            for h in range(8):
                def post(sg, t0, tn, h=h):
                    for rc in range(2):
                        bk = bank()
                        p.mm(bk[:, :tn], wukT[:, h, rc * 128:(rc + 1) * 128], sg[:, :tn])
                        s2_ = stg[si[0] % 4]; si[0] += 1
                        p.act(s2_[:, :tn], bk[:, :tn], AF.Copy, scale=128.0 ** -0.5)
                        r0 = (h * 2 + rc) * 128
                        p.dma("sp", FT["qlT"][r0:r0 + 128, t0:t0 + tn], s2_[:, :tn])
                fm_group(O_AQ + h * 128, 128, None, 0, real_only=True, post=post)
            for c in range(2):
                fm_group(O_CKV + c * 128, 128, FT["ckvT"], c * 128)
            for c in range(8):
                fm_group(O_IQ + c * 128, 128, FT["iqT"], c * 128, real_only=True)
            fm_group(O_IK, 128, FT["ikT"], 0, dup=True)
            for c in range(4):
                fm_group(O_GQ + c * 128, 128, FT["gqT"], c * 128, real_only=True)
            for c in range(4):
                fm_group(O_GK + c * 128, 128, FT["gkT"], c * 128)
            fm_group(O_GR, 16, FT["grT"], 0)

            with p.scope() as s3:
                wtm = [p.sb(s3, "wtm%d" % i, [128, 16, 512], BF) for i in range(2)]
                k = 0
                for (col0, ncols, dst, dcol0, real_only) in (
                        (O_CKV, 256, TM["ckv"], 0, False),
                        (O_IW, 16, TM["iw"], 0, True),
                        (O_GV, 512, TM["gv"], 0, False), (O_GV + 512, 512, TM["gv"], 512, False),
                        (O_GO, 512, TM["go"], 0, True), (O_GO + 512, 512, TM["go"], 512, True)):
                    w = wtm[k % 2]; k += 1
                    p.dma("pool", w[:, :, :ncols], V(w_in, w_in_r[:, :, col0:col0 + ncols]))
                    for ti in range(NT + 1):
                        if real_only and ti == 0:
                            continue
                        n = NM if ti == 0 else 128
                        c0 = 0 if ti == 0 else NM + (ti - 1) * 128
                        bk = bank()
                        ht_, sl_ = hT_cols(c0, n)
                        for c in range(16):
                            p.mm(bk[:n, :ncols], ht_[:, c, sl_], w[:, c, :ncols],
                                 start=(c == 0), stop=(c == 15))
                        if dst is TM["iw"]:
                            sg = stgf[si[0] % 2]; si[0] += 1
                            p.copy("dve", sg[:n, :ncols], bk[:n, :ncols])
                        else:
                            sg = stg[si[0] % 4]; si[0] += 1
                            if si[0] % 2:
                                p.copy("act", sg[:n, :ncols], bk[:n, :ncols])
                            else:
                                p.copy("dve", sg[:n, :ncols], bk[:n, :ncols])
                        p.dma("sp", dst[c0:c0 + n, dcol0:dcol0 + ncols], sg[:n, :ncols])
        p.barrier()
        p.release(*sA.tiles)
        sA.close()

        if stop_after == "B":
            p.barrier()
            return nc, dbg

        def bank6():
            b = banks[p.bank_i % 6]
            p.bank_i += 1
            return b
        ob_i = [0]

        def obank():
            ob_i[0] += 1
            return banks[6 + ob_i[0] % 2]

        sY = contextlib.ExitStack()
        sY.tiles = []
        es.enter_context(sY)
        yT = p.sb(sY, "yT", [128, 16, S], BF)
        yTd = scratch("yTd", [128, 16 * S], BF)

        Etab = scratch("Etab", [8, 512], F32)
        if debug and "scoreD" in debug:
            scratch("scoreD", [128, L], F32)
            scratch("thrD", [128, 1], F32)
            scratch("olD", [128, 2048], BF)
            scratch("maskTD", [128, 17 * 128], BF)
        with p.scope() as sc:
            ikT = p.sb(sc, "ikT_s", [128, L], BF)
            ckvT = p.sb(sc, "ckvT_s", [128, 2, L], BF)
            ckva = p.sb(sc, "ckva", [128, 17, 257], BF)
            wuv = p.sb(sc, "wuv", [128, 16, 128], BF)
            BT = p.sb(sc, "BT", [128, 8, 4, 128], BF)
            p.dma("sp", ikT.v, FT["ikT"].v)
            p.dma("sp", ckvT.v, V(FT["ckvT"], FT["ckvT"].h.ap().rearrange("(c p) t -> p c t", p=128)))
            p.memset("dve", ckva.v, 1.0)
            p.dma("sp", ckva[:NM, 0, 0:256], TM["ckv"][0:NM, :])
            p.dma("sp", ckva[:, 1:17, 0:256],
                  V(TM["ckv"], TM["ckv"].h[NM:L, :].rearrange("(t p) r -> p t r", p=128)))
            p.dma("pool", wuv.v, V(w_uv, w_uv.h.ap().rearrange("h (rc p) d -> p (h rc) d", p=128)))
            with p.scope() as sb_:
                rb = p.sb(sb_, "rb", [32, 8], F32)
                boh = p.sb(sb_, "boh", [32, 512], F32)
                es_ = p.sb(sb_, "es_", [8, 512], F32)
                p.dma("sp", rb.v, rel_bias.v)
                p.dma("sp", boh.v, boh_in.v)
                bk = bank6()
                p.mm(bk[:8, :], rb.v, boh.v)
                p.copy("dve", es_.v, bk[:8, :])
                p.dma("sp", Etab.v, es_.v)
            for h in range(8):
                for kind, (off, ncol, ps) in enumerate(((128, 128, 1), (0, 128, 1), (0, 128, 0), (112, NM, 1))):
                    src = bass.AP(tensor=Etab.h, offset=h * 512 + off, ap=[[ps, 128], [1, ncol]])
                    p.dma("pool", BT[:, h, kind, :ncol], V(Etab, src))

            iqs = [p.sb(sc, "iqs%d" % i, [128, 8, 128], BF) for i in range(2)]
            qls = [p.sb(sc, "qls%d" % i, [128, 16, 128], BF) for i in range(3)]
            iws = [p.sb(sc, "iws%d" % i, [128, 16], F32) for i in range(2)]
            scores_ = [p.sb(sc, "score%d" % i, [128, L], F32) for i in range(2)]
            rl = [p.sb(sc, "rl%d" % i, [128, 512], F32) for i in range(4)]
            NB = 16
            ck = p.sb(sc, "ck", [128, NB], F32)
            for k in range(NB):
                p.memset("pool", ck[:, k:k + 1], 0.5 ** (k + 1))
            bsts = [p.sb(sc, "bst%d" % i, [128, 8], F32) for i in range(2)]
            nRks = [p.sb(sc, "nRk%d" % i, [128, NB], F32) for i in range(2)]
            cnts = [p.sb(sc, "cnt%d" % i, [128, NB], F32) for i in range(2)]
            sjunk = p.sb(sc, "sjunk", [128, L], BF)
            thr = p.sb(sc, "thr", [128, 1], F32)
            maskq = p.sb(sc, "maskq", [128, L], BF)
            maskTs = [p.sb(sc, "maskT%d" % i, [128, 17, 128], BF) for i in range(2)]
            PTs = [p.sb(sc, "PT%d" % i, [128, 512], BF) for i in range(4)]
            rden = p.sb(sc, "rden", [128, 16], F32)
            olsb = p.sb(sc, "olsb", [128, 8, 256], BF)
            olT = p.sb(sc, "olT", [128, 16, 128], BF)
            iqT_r = FT["iqT"].h.ap().rearrange("(c p) t -> p c t", p=128)
            qlT_r = FT["qlT"].h.ap().rearrange("(c p) t -> p c t", p=128)
            rli = [0]; pti = [0]

            def tiles_of(tq):
                return [(0, 0, NM)] + [(i + 1, NM + 128 * i, 128) for i in range(tq + 1)]

            def bisect_steps(tq):
                Wk = NM + 128 * (tq + 1)
                if Wk <= 256:
                    return
                score = scores_[tq % 2]; bst = bsts[tq % 2]; nRk = nRks[tq % 2]; cnt = cnts[tq % 2]
                for k in range(NB):
                    p.ts("dve", sjunk[:, :Wk], score[:, :Wk], bst[:, 4:5], 0.0, ALU.is_ge, ALU.add,
                         accum=cnt[:, k:k + 1])
                    p.stt("dve", bst[:, 5:6], cnt[:, k:k + 1], 256.0, nRk[:, k:k + 1], ALU.is_ge, ALU.mult)
                    kn_ = k + 1 if k + 1 < NB else k
                    p.stt("dve", bst[:, 4:5], bst[:, 4:5], nRk[:, kn_:kn_ + 1], bst[:, 5:6], ALU.subtract, ALU.add)
                    yield

            def scores(tq, steps):
                q0 = NM + tq * 128
                Wk = NM + 128 * (tq + 1)
                iq = iqs[tq % 2]; ql = qls[tq % 3]; iw = iws[tq % 2]
                score = scores_[tq % 2]; bst = bsts[tq % 2]; nRk = nRks[tq % 2]; cnt = cnts[tq % 2]
                p.dma("sp", iq.v, V(FT["iqT"], iqT_r[:, :, q0:q0 + 128]))
                p.dma("sp", ql.v, V(FT["qlT"], qlT_r[:, :, q0:q0 + 128]))
                p.dma("sp", iw.v, TM["iw"][q0:q0 + 128, :])
                nblk = len(range(0, Wk, 512))
                for bi, k0 in enumerate(range(0, Wk, 512)):
                    kn = min(512, Wk - k0)
                    for h in range(16):
                        bk = bank6()
                        pb = (h % 2) * 64
                        p.mm(bk[:, :kn], iq[pb:pb + 64, h // 2, :], ikT[pb:pb + 64, k0:k0 + kn])
                        r = rl[rli[0] % 4]; rli[0] += 1
                        p.act(r[:, :kn], bk[:, :kn], AF.Relu)
                        if h == 0:
                            p.ts("dve", score[:, k0:k0 + kn], r[:, :kn], iw[:, 0:1], None, ALU.mult)
                        else:
                            p.stt("dve", score[:, k0:k0 + kn], r[:, :kn], iw[:, h:h + 1],
                                  score[:, k0:k0 + kn], ALU.mult, ALU.add)
                        if h % 4 == 3 and steps is not None:
                            next(steps, None)
                if steps is not None:
                    for _ in steps:
                        pass
                if Wk > 256:
                    p.op("dve", [score.v], [bst.v],
                         lambda e: e.tensor_reduce(bst[:, 0:1].ap, score[:, :Wk].ap, AX.X, ALU.max))
                    p.op("dve", [score.v], [bst.v],
                         lambda e: e.tensor_reduce(bst[:, 1:2].ap, score[:, :Wk].ap, AX.X, ALU.min))
                    p.tt("dve", bst[:, 2:3], bst[:, 0:1], bst[:, 1:2], ALU.subtract)
                    p.ts("dve", bst[:, 2:3], bst[:, 2:3], 2.0, None, ALU.add)
                    p.ts("dve", nRk.v, ck.v, bst[:, 2:3], None, ALU.mult)
                    p.stt("dve", bst[:, 4:5], bst[:, 1:2], -1.0, nRk[:, 0:1], ALU.add, ALU.add)
                p.memset("dve", score[0:64, Wk - 64:Wk], NEG)

            def finalize(tq):
                Wk = NM + 128 * (tq + 1)
                score = scores_[tq % 2]; bst = bsts[tq % 2]
                if Wk > 256:
                    p.ts("dve", thr.v, bst[:, 4:5], -1.0e29, None, ALU.max)
                else:
                    p.memset("dve", thr.v, -1.0e29)
                p.ts("dve", maskq[:, :Wk], score[:, :Wk], thr[:, 0:1], None, ALU.is_ge)

            def mask_transposes(tq):
                maskT = maskTs[tq % 2]
                tiles = tiles_of(tq)
                for g0 in range(0, len(tiles), 8):
                    grp = tiles[g0:g0 + 8]
                    bk = bank6(); bv = bk.v.bitcast(BF)
                    for j, (ti, c0, kn) in enumerate(grp):
                        p.tr(bv[:kn, j * 128:(j + 1) * 128], maskq[:, c0:c0 + kn], ident_b.v)
                    if grp[0][0] == 0:
                        p.copy("act", maskT[:NM, 0, :], bv[:NM, 0:128])
                        if len(grp) > 1:
                            p.copy("act", maskT[:, 1:len(grp), :].rearrange("p t q -> p (t q)"),
                                   bv[:, 128:128 * len(grp)])
                    else:
                        p.copy("act", maskT[:, grp[0][0]:grp[0][0] + len(grp), :].rearrange("p t q -> p (t q)"),
                               bv[:, 0:128 * len(grp)])

            def attention(tq):
                maskT = maskTs[tq % 2]
                ql = qls[tq % 3]
                tiles = tiles_of(tq)
                pend = []

                def flush(item):
                    h, ob, PT, grp, g0, first_ = item
                    for j, (ti, c0, kn) in enumerate(grp):
                        last = (g0 + j == len(tiles) - 1)
                        p.mm(ob[:, 0:257], PT[:kn, j * 128:(j + 1) * 128], ckva[:kn, ti, :],
                             start=(first_ and j == 0), stop=last)
                    if g0 + len(grp) == len(tiles):
                        p.act(rden[:, 8 + h:9 + h], ob[:, 256:257], AF.Ln)
                        p.act(rden[:, h:h + 1], rden[:, 8 + h:9 + h], AF.Exp, scale=-1.0)
                        p.act(olsb[:, h, :], ob[:, 0:256], AF.Copy, scale=rden[:, h:h + 1])

                for h in range(8):
                    ob = obank()
                    for g0 in range(0, len(tiles), 4):
                        grp = tiles[g0:g0 + 4]
                        bk = bank6()
                        for j, (ti, c0, kn) in enumerate(grp):
                            if ti == 0:
                                kind = 3 if tq == 0 else 2
                            else:
                                dlt = tq - (ti - 1)
                                kind = 0 if dlt == 0 else (1 if dlt == 1 else 2)
                            o_ = bk[:kn, j * 128:(j + 1) * 128]
                            p.mm(o_, ckvT[:, 0, c0:c0 + kn], ql[:, h * 2, :], start=True, stop=False)
                            p.mm(o_, ckvT[:, 1, c0:c0 + kn], ql[:, h * 2 + 1, :], start=False, stop=False)
                            p.mm(o_, BT[:, h, kind, :kn], jrev_b.v, start=False, stop=True)
                        PT = PTs[pti[0] % 4]; pti[0] += 1
                        w_ = 128 * len(grp)
                        if grp[0][0] == 0:
                            p.act(PT[:NM, 0:128], bk[:NM, 0:128], AF.Exp)
                            p.tt("pool", PT[:NM, 0:128], PT[:NM, 0:128], maskT[:NM, 0, :], ALU.mult)
                            if len(grp) > 1:
                                p.act(PT[:, 128:w_], bk[:, 128:w_], AF.Exp)
                                p.tt("pool", PT[:, 128:w_], PT[:, 128:w_],
                                     maskT[:, 1:len(grp), :].rearrange("p t q -> p (t q)"), ALU.mult)
                        else:
                            p.act(PT[:, 0:w_], bk[:, 0:w_], AF.Exp)
                            p.tt("pool", PT[:, 0:w_], PT[:, 0:w_],
                                 maskT[:, grp[0][0]:grp[0][0] + len(grp), :].rearrange("p t q -> p (t q)"),
                                 ALU.mult)
                        pend.append((h, ob, PT, grp, g0, g0 == 0))
                        if len(pend) > 2:
                            flush(pend.pop(0))
                while pend:
                    flush(pend.pop(0))
                for g in range(2):
                    bk = bank6(); bv = bk.v.bitcast(BF)
                    for j in range(8):
                        hr = g * 8 + j
                        p.tr(bv[:, j * 128:(j + 1) * 128], olsb[:, hr // 2, (hr % 2) * 128:(hr % 2 + 1) * 128],
                             ident_b.v)
                    p.copy("act", olT[:, g * 8:(g + 1) * 8, :].rearrange("p c q -> p (c q)"), bv)
                for g in range(2):
                    bk = bank6()
                    for j in range(4):
                        h = g * 4 + j
                        for rc in range(2):
                            p.mm(bk[:, j * 128:(j + 1) * 128], wuv[:, h * 2 + rc, :], olT[:, h * 2 + rc, :],
                                 start=(rc == 0), stop=(rc == 1))
                    p.copy("act", yT[:, g * 4:(g + 1) * 4, tq * 128:(tq + 1) * 128],
                           bk.v.rearrange("p (c q) -> p c q", q=128))

            for it_ in range(NT + 2):
                steps = bisect_steps(it_ - 1) if 1 <= it_ <= NT else None
                if it_ < NT:
                    scores(it_, steps)
                elif steps is not None:
                    for _ in steps:
                        pass
                if 1 <= it_ <= NT:
                    finalize(it_ - 1)
                if it_ >= 2:
                    attention(it_ - 2)
                if 1 <= it_ <= NT:
                    mask_transposes(it_ - 1)
        if stop_after == "C1":
            p.dma("sp", yTd.v, yT.v.rearrange("p c t -> p (c t)"))
            p.barrier()
            return nc, dbg

        sW = contextlib.ExitStack()
        sW.tiles = []
        es.enter_context(sW)
        wout = p.sb(sW, "wout", [128, 16, D], BF)
        w_out_r = w_out.h.ap().rearrange("(c p) n -> p c n", p=128)
        for j in range(4):
            p.dma("pool", wout[:, :, j * 512:(j + 1) * 512], V(w_out, w_out_r[:, :, j * 512:(j + 1) * 512]))
        gB1 = p.sb(sW, "gB1", [128, D], F32)
        bB1 = p.sb(sW, "bB1", [128, D], F32)
        p.dma("sp", gB1.v, V(ln1_g, ln1_g.h.ap().partition_broadcast(128)))
        p.dma("sp", bB1.v, V(ln1_b, ln1_b.h.ap().partition_broadcast(128)))
        with p.scope() as sg_:
            S_f = p.sb(sg_, "S_f", [128, 4, 256], F32)
            S_b = p.sb(sg_, "S_b", [128, 4, 256], BF)
            wgk = p.sb(sg_, "wgk", [16, 512], BF)
            bgk = p.sb(sg_, "bgk", [1, 512], BF)
            ones1 = p.sb(sg_, "ones1", [1, 128], BF)
            gng4 = p.sb(sg_, "gng4", [128, 4, 256], F32)
            p.memset("dve", S_f.v, 0.0)
            p.memset("dve", S_b.v, 0.0)
            p.memset("dve", ones1.v, 1.0)
            p.dma("pool", wgk.v, w_gk2.v)
            p.dma("pool", bgk.v, b_gk.v)
            for hh in range(4):
                p.dma("sp", gng4[:, hh, :], V(gla_g, gla_g.h.ap().partition_broadcast(128)))
            grs = [p.sb(sg_, "grs%d" % i, [16, 128], BF) for i in range(2)]
            gqs = [p.sb(sg_, "gqs%d" % i, [128, 4, 128], BF) for i in range(2)]
            gks = [p.sb(sg_, "gks%d" % i, [128, 4, 128], BF) for i in range(2)]
            vts = [p.sb(sg_, "vts%d" % i, [128, 1024], BF) for i in range(2)]
            gos = [p.sb(sg_, "gos%d" % i, [128, 1024], BF) for i in range(2)]
            lpe = p.sb(sg_, "lpe", [128, 512], F32)
            lp = p.sb(sg_, "lp", [128, 512], F32)
            E1 = p.sb(sg_, "E1", [128, 4, 128], F32)
            E2 = p.sb(sg_, "E2", [128, 4, 128], F32)
            qtl = p.sb(sg_, "qtl", [128, 4, 128], BF)
            ktl = p.sb(sg_, "ktl", [128, 4, 128], BF)
            ktok = p.sb(sg_, "ktok", [128, 4, 128], BF)
            ATs = p.sb(sg_, "ATs", [128, 4, 128], BF)
            tmpS = p.sb(sg_, "tmpS", [128, 256], F32)
            ssq = p.sb(sg_, "ssq", [128, 8], F32)
            gsil = p.sb(sg_, "gsil", [128, 4, 256], F32)
            ybt = p.sb(sg_, "ybt", [128, 1024], BF)
            junkg = p.sb(sg_, "junkg", [128, 256], BF)
            gqT_r = FT["gqT"].h.ap().rearrange("(c p) t -> p c t", p=128)
            gkT_r = FT["gkT"].h.ap().rearrange("(c p) t -> p c t", p=128)
            for ti in range(NT + 1):
                n = NM if ti == 0 else 128
                c0 = 0 if ti == 0 else NM + (ti - 1) * 128
                real = ti > 0
                gr = grs[ti % 2]; gq = gqs[ti % 2]; gk_ = gks[ti % 2]; vt = vts[ti % 2]; go = gos[ti % 2]
                p.dma("sp", gr[:, :n], FT["grT"][:, c0:c0 + n])
                p.dma("sp", gk_[:, :, :n], V(FT["gkT"], gkT_r[:, :, c0:c0 + n]))
                p.dma("sp", vt[:n, :], TM["gv"][c0:c0 + n, :])
                if real:
                    p.dma("sp", gq[:, :, :n], V(FT["gqT"], gqT_r[:, :, c0:c0 + n]))
                    p.dma("sp", go[:n, :], TM["go"][c0:c0 + n, :])
                bA = bank6()
                p.mm(bA[:n, :], gr[:, :n], wgk.v, start=True, stop=False)
                p.mm(bA[:n, :], ones1[:, :n], bgk.v, start=False, stop=True)
                p.act(lpe[:n, :], bA[:n, :], AF.Exp, scale=-1.0)
                p.act(lp[:n, :], lpe[:n, :], AF.Ln, bias=1.0)
                bB_ = bank6()
                for hh in range(4):
                    p.mm(bB_[:, hh * 128: hh * 128 + n], lp[:n, hh * 128:(hh + 1) * 128], tri_f[:n, :n])
                bBv = bB_.v.rearrange("p (h i) -> p h i", i=128)[:, :, :n]
                p.act(E1[:, :, :n], bBv, AF.Exp, scale=-1.0 / 16.0)
                p.act(E2[:, :, :n], bBv, AF.Exp, scale=1.0 / 16.0)
                p.tt("dve", ktl[:, :, :n], gk_[:, :, :n], E2[:, :, :n], ALU.mult)
                bT_ = bank6(); bTv = bT_.v.bitcast(BF)
                for hh in range(4):
                    p.tr(bTv[:n, hh * 128:(hh + 1) * 128], ktl[:, hh, :n], ident_b.v)
                p.copy("act", ktok[:n, :, :].rearrange("p h d -> p (h d)"), bTv[:n, 0:512])
                if real:
                    p.stt("dve", qtl[:, :, :n], gq[:, :, :n], 128.0 ** -0.5, E1[:, :, :n], ALU.mult, ALU.mult)
                    bC = bank6()
                    for hh in range(4):
                        p.mm(bC[:n, hh * 128: hh * 128 + n], ktl[:, hh, :n], qtl[:, hh, :n])
                    for hh in range(4):
                        p.tt("dve", ATs[:n, hh, :n], bC[:n, hh * 128: hh * 128 + n], tri_b[:n, :n], ALU.mult)
                    bO = [bank6(), bank6()]
                    for hh in range(4):
                        o_ = bO[hh // 2][:, (hh % 2) * 256:(hh % 2 + 1) * 256]
                        p.mm(o_, qtl[:, hh, :n], S_b[:, hh, :], start=True, stop=False)
                        p.mm(o_, ATs[:n, hh, :n], vt[:n, hh * 256:(hh + 1) * 256], start=False, stop=True)
                for hh in range(4):
                    bS = bank6()
                    p.mm(bS[:, 0:256], ktok[:n, hh, :], vt[:n, hh * 256:(hh + 1) * 256])
                    p.ts("dve", tmpS.v, bS[:, 0:256], E1[:, hh, n - 1:n], None, ALU.mult)
                    p.stt("dve", S_f[:, hh, :], S_f[:, hh, :], E1[:, hh, n - 1:n], tmpS.v, ALU.mult, ALU.add)
                p.copy("act", S_b.v, S_f.v)
                if real:
                    p.memset("dve", ssq[:, 0:4], 0.0)
                    for hh in range(4):
                        o_ = bO[hh // 2][:, (hh % 2) * 256:(hh % 2 + 1) * 256]
                        p.act(junkg.v, o_, AF.Square, accum=ssq[:, hh:hh + 1])
                    p.ts("dve", ssq[:, 4:8], ssq[:, 0:4], 1.0 / 256.0, EPS, ALU.mult, ALU.add)
                    p.act(ssq[:, 4:8], ssq[:, 4:8], AF.Sqrt)
                    p.recip(ssq[:, 4:8], ssq[:, 4:8])
                    p.act(gsil.v.rearrange("p h v -> p (h v)"), go.v, AF.Silu)
                    p.tt("dve", gsil.v, gsil.v, gng4.v, ALU.mult)
                    for hh in range(4):
                        o_ = bO[hh // 2][:, (hh % 2) * 256:(hh % 2 + 1) * 256]
                        p.stt("dve", ybt[:, hh * 256:(hh + 1) * 256], o_, ssq[:, 4 + hh:5 + hh], gsil[:, hh, :],
                              ALU.mult, ALU.mult)
                    bY = bank6(); bYv = bY.v.bitcast(BF)
                    for c in range(8):
                        p.tr(bYv[:, c * 128:(c + 1) * 128], ybt[:, c * 128:(c + 1) * 128], ident_b.v)
                    p.copy("act", yT[:, 8:16, (ti - 1) * 128: ti * 128], bYv.rearrange("p (c t) -> p c t", t=128))

        if stop_after == "C2":
            p.dma("sp", yTd.v, yT.v.rearrange("p c t -> p (c t)"))
            p.barrier()
            return nc, dbg

        with p.scope() as sd:
            hts = [p.sb(sd, "hts%d" % i, [128, D], F32) for i in range(2)]
            zb1 = p.sb(sd, "zb1", [128, D], BF)
            junkd = p.sb(sd, "junkd", [128, D], BF)
            std = p.sb(sd, "std", [128, 8], F32)
            junkd2 = p.sb(sd, "junkd2", [128, D], BF)
            std2 = p.sb(sd, "std2", [128, 8], F32)

            def d_mm(t):
                p.dma("sp", hts[t % 2].v, hd[t * 128:(t + 1) * 128, :])
                for j in range(4):
                    bk = banks[(t % 2) * 4 + j]
                    for c in range(16):
                        p.mm(bk.v, yT[:, c, t * 128:(t + 1) * 128], wout[:, c, j * 512:(j + 1) * 512],
                             start=(c == 0), stop=(c == 15))

            def d_res(t):
                ht = hts[t % 2]
                for j in range(4):
                    p.stt("dve", ht[:, j * 512:(j + 1) * 512], ht[:, j * 512:(j + 1) * 512], ALPHA,
                          banks[(t % 2) * 4 + j].v, ALU.mult, ALU.add)

            def d_ln(t):
                ht = hts[t % 2]
                layer_norm(128, ht.v, zb1.v, gB1.v, bB1.v, std if t % 2 else std2, junkd.v if t % 2 else junkd2.v)
                p.dma("sp", h1d[t * 128:(t + 1) * 128, :], ht.v)
                for half in range(2):
                    bk = banks[(t % 2) * 4 + half]; bv = bk.v.bitcast(BF)
                    for c in range(8):
                        cc = half * 8 + c
                        p.tr(bv[:, c * 128:(c + 1) * 128], zb1[:, cc * 128:(cc + 1) * 128], ident_b.v)
                    p.copy("act", yT[:, half * 8:(half + 1) * 8, t * 128:(t + 1) * 128],
                           bv.rearrange("p (c t) -> p c t", t=128))

            d_mm(0)
            d_res(0)
            for t in range(NT):
                if t + 1 < NT:
                    d_mm(t + 1)
                d_ln(t)
                if t + 1 < NT:
                    d_res(t + 1)
        p.barrier()
        p.release(*sW.tiles)
        sW.close()
        if stop_after == "D":
            p.barrier()
            return nc, dbg

        h1T = yT
        sD = scratch("sD", [S, 2048], F32)
        with p.scope() as s1_:
            qhT = p.sb(s1_, "qhT", [128, 16, S], BF)
            skn = p.sb(s1_, "skn", [128, 16, 128], BF)
            skT = p.sb(s1_, "skT", [128, 16, 128], BF)
            p.dma("pool", skn.v, V(sub_keys, sub_keys.h.ap().rearrange("a n c -> n a c")))
            for g in range(2):
                bk = bank(); bv = bk.v.bitcast(BF)
                for j in range(8):
                    p.tr(bv[:, j * 128:(j + 1) * 128], skn[:, g * 8 + j, :], ident_b.v)
                p.copy("act", skT[:, g * 8:(g + 1) * 8, :].rearrange("p a n -> p (a n)"), bv)
            wps = [p.sb(s1_, "wps%d" % i, [128, 16, 128], BF) for i in range(3)]
            w_pq_r = w_pq.h.ap().rearrange("(c p) n -> p c n", p=128)
            for hs in range(16):
                w = wps[hs % 3]
                p.dma("pool", w.v, V(w_pq, w_pq_r[:, :, hs * 128:(hs + 1) * 128]))
                for tb in range(4):
                    bk = bank()
                    for c in range(16):
                        p.mm(bk.v, w[:, c, :], h1T[:, c, tb * 512:(tb + 1) * 512], start=(c == 0), stop=(c == 15))
                    if (hs + tb) % 2:
                        p.copy("act", qhT[:, hs, tb * 512:(tb + 1) * 512], bk.v)
                    else:
                        p.copy("dve", qhT[:, hs, tb * 512:(tb + 1) * 512], bk.v)
            sst = [p.sb(s1_, "sst%d" % i, [128, 2048], F32) for i in range(2)]
            for t in range(NT):
                ss_ = sst[t % 2]
                for g in range(4):
                    bk = bank()
                    for j in range(4):
                        hs = g * 4 + j
                        p.mm(bk[:, j * 128:(j + 1) * 128], qhT[:, hs, t * 128:(t + 1) * 128], skT[:, hs, :])
                    if g % 2:
                        p.copy("act", ss_[:, g * 512:(g + 1) * 512], bk.v)
                    else:
                        p.copy("dve", ss_[:, g * 512:(g + 1) * 512], bk.v)
                p.dma("sp", sD[t * 128:(t + 1) * 128, :], ss_.v)
        if stop_after == "P1":
            p.barrier()
            return nc, dbg
        h1Td = scratch("h1Td", [128, 16 * S], BF)
        p.dma("sp", h1Td.v, yT.v.rearrange("p c t -> p (c t)"))
        p.barrier()
        p.release(yT)
        sY.close()
        GT3 = scratch("GT3", [NT, 128, 128 * 128], BF)
        with p.scope() as s2_:
            sts = [p.sb(s2_, "sts%d" % i, [128, 16, 128], F32) for i in range(2)]
            wa = p.sb(s2_, "wa", [128, 128], F32)
            t16 = p.sb(s2_, "t16", [128, 8, 2, 16], F32)
            cand = p.sb(s2_, "cand", [128, 256], F32)
            cw = p.sb(s2_, "cw", [128, 256], F32)
            c16 = p.sb(s2_, "c16", [128, 8, 16], F32)
            exs = p.sb(s2_, "exs", [128, 8, 16], F32)
            sm = p.sb(s2_, "sm", [128, 4, 8], F32)
            theta = p.sb(s2_, "theta", [128, 8, 16], F32)
            E_ = p.sb(s2_, "E_", [128, 16, 128], F32)
            OAfs = [p.sb(s2_, "OAf%d" % i, [128, 128, 64], BF) for i in range(2)]
            OBfs = [p.sb(s2_, "OBf%d" % i, [128, 128, 64], BF) for i in range(2)]
            OAp = p.sb(s2_, "OAp", [128, 128, 128], BF)
            OBp = p.sb(s2_, "OBp", [128, 128, 128], BF)
            Gs = p.sb(s2_, "Gs", [128, 128, 128], BF)
            ev = [0]
            for t in range(NT):
                st_ = sts[t % 2]
                if t == 0:
                    p.dma("sp", st_.v.rearrange("p a n -> p (a n)"), sD[0:128, :])
                if t + 1 < NT:
                    p.dma("sp", sts[(t + 1) % 2].v.rearrange("p a n -> p (a n)"), sD[(t + 1) * 128:(t + 2) * 128, :])
                st4 = st_.v.rearrange("p (h s) n -> p h s n", s=2)
                for hh in range(8):
                    for sd_ in range(2):
                        sv = st_[:, hh * 2 + sd_, :]
                        p.max8(t16[:, hh, sd_, 0:8], sv)
                        p.mrep(wa.v, t16[:, hh, sd_, 0:8], sv, -3.0e38)
                        p.max8(t16[:, hh, sd_, 8:16], wa.v)
                    cv = cand.v.rearrange("p (a b) -> p a b", b=16)
                    in0 = V(t16, t16.h[:, hh, 0, :].unsqueeze(2).to_broadcast([128, 16, 16]))
                    in1 = V(t16, t16.h[:, hh, 1, :].unsqueeze(1).to_broadcast([128, 16, 16]))
                    p.tt("dve", cv, in0, in1, ALU.add)
                    p.max8(c16[:, hh, 0:8], cand.v)
                    p.mrep(cw.v, c16[:, hh, 0:8], cand.v, -3.0e38)
                    p.max8(c16[:, hh, 8:16], cw.v)
                p.tt("dve", exs.v, c16.v, V(c16, c16.h[:, :, 0:1].to_broadcast([128, 8, 16])), ALU.subtract)
                p.act(exs.v, exs.v, AF.Exp)
                p.rsum("dve", sm[:, 0, :], exs.v)
                p.recip(sm[:, 1, :], sm[:, 0, :])
                p.ts("dve", sm[:, 2, :], c16[:, :, 15], -2.0e-5, None, ALU.add)
                p.tt("dve", theta.v, V(sm, sm.h[:, 2, :].unsqueeze(2).to_broadcast([128, 8, 16])), t16[:, :, 0, :],
                     ALU.subtract)
                m16 = t16.h[:, :, :, 0:1].rearrange("p h s o -> p (h s) o").to_broadcast([128, 16, 128])
                p.tt("dve", E_.v, st_.v, V(t16, m16), ALU.subtract)
                p.act(E_.v, E_.v, AF.Exp)
                E4 = E_.v.rearrange("p (h s) n -> p h s n", s=2)
                p.tt("dve", E4[:, :, 0, :], E4[:, :, 0, :],
                     V(sm, sm.h[:, 1, :].unsqueeze(2).to_broadcast([128, 8, 128])), ALU.mult)
                for hf_ in range(2):
                    OAf = OAfs[hf_]; OBf = OBfs[hf_]
                    isl = slice(hf_ * 64, (hf_ + 1) * 64)
                    for qd in range(4):
                        hs_ = slice(qd * 2, qd * 2 + 2)
                        OA = V(OAf, OAf.h[:, qd * 32:(qd + 1) * 32, :].rearrange("t (h a) i -> t h a i", h=2))
                        OB = V(OBf, OBf.h[:, qd * 32:(qd + 1) * 32, :].rearrange("t (h a) i -> t h a i", h=2))
                        sa_b = V(st_, st4.ap[:, hs_, 0, isl].unsqueeze(2).to_broadcast([128, 2, 16, 64]))
                        sb_b = V(st_, st4.ap[:, hs_, 1, isl].unsqueeze(2).to_broadcast([128, 2, 16, 64]))
                        s1_b = V(t16, t16.h[:, hs_, 0, :].unsqueeze(3).to_broadcast([128, 2, 16, 64]))
                        th_b = V(theta, theta.h[:, hs_, :].unsqueeze(3).to_broadcast([128, 2, 16, 64]))
                        ea_b = V(E_, E4.ap[:, hs_, 0, isl].unsqueeze(2).to_broadcast([128, 2, 16, 64]))
                        eb_b = V(E_, E4.ap[:, hs_, 1, isl].unsqueeze(2).to_broadcast([128, 2, 16, 64]))
                        p.tt("dve", OA, sa_b, s1_b, ALU.is_equal)
                        p.tt("dve", OA, OA, ea_b, ALU.mult)
                        p.tt("dve", OB, sb_b, th_b, ALU.is_ge)
                        p.tt("dve", OB, OB, eb_b, ALU.mult)
                    for (src, dstp) in ((OAf, OAp), (OBf, OBp)):
                        for ig in range(8):
                            bk = bank(); bv = bk.v.bitcast(BF)
                            for j in range(8):
                                p.tr(bv[:, j * 128:(j + 1) * 128], src[:, :, ig * 8 + j], ident_b.v)
                            i0_ = hf_ * 64 + ig * 8
                            p.copy("act", dstp[:, i0_:i0_ + 8, :].rearrange("p i t -> p (i t)"), bv)
                for tl in range(128):
                    if tl % 4 == 0:
                        bk = bank()
                        bkv = bk.v.rearrange("p (j f) -> p j f", f=4)
                    p.mm(bkv[:, :, tl % 4], OAp[:, :, tl], OBp[:, :, tl])
                    if tl % 4 == 3:
                        ev[0] += 1
                        p.copy("act", Gs[:, :, tl - 3:tl + 1], bkv)
                p.dma("act", GT3[t], Gs.v.rearrange("p j t -> p (j t)"))
        if stop_after == "P2":
            p.barrier()
            return nc, dbg
        IG = 4
        u_r = u_tab.h.ap().rearrange("(i j) d -> j i d", j=128)
        v_r = v_tab.h.ap().rearrange("(i j) d -> j i d", j=128)
        sH = contextlib.ExitStack()
        sH.tiles = []
        es.enter_context(sH)
        h1T = p.sb(sH, "h1T", [128, 16, S], BF)
        p.dma("sp", h1T.v.rearrange("p c t -> p (c t)"), h1Td.v)
        for hf in range(2):
            with p.scope() as s3_:
                acc = p.sb(s3_, "acc", [128, 8, D], F32)
                with p.scope() as s4_:
                    urows = [p.sb(s4_, "urow%d" % i, [128, D], BF) for i in range(3)]
                    uTs = [p.sb(s4_, "uT%d" % i, [128, 16, 128], BF) for i in range(2)]
                    gti = [p.sb(s4_, "gti%d" % i, [128, 1024], BF) for i in range(2)]
                    gel = [p.sb(s4_, "gel%d" % i, [128, 512], BF) for i in range(2)]
                    GHs = [p.sb(s4_, "GH%d" % i, [128, IG, 1024], BF) for i in range(2)]
                    vss = [p.sb(s4_, "vs%d" % i, [128, IG, D], BF) for i in range(2)]
                    gi = [0]

                    def load(i_):
                        ur = urows[i_ % 3]
                        vs = vss[(i_ // IG) % 2]
                        p.dma("pool", ur.v, V(u_tab, u_r[i_]))
                        p.dma("pool", vs[:, i_ % IG, :], V(v_tab, v_r[i_]))

                    def prep(i_):
                        ur = urows[i_ % 3]; uT = uTs[i_ % 2]; gt = gti[i_ % 2]
                        src_g = GT3.h[hf * 8:(hf + 1) * 8, :, i_ * 128:(i_ + 1) * 128].rearrange("a i t -> i a t")
                        p.dma("sp", gt.v.rearrange("p (a t) -> p a t", t=128), V(GT3, src_g))
                        for g in range(2):
                            bk = bank(); bv = bk.v.bitcast(BF)
                            for j in range(8):
                                c = g * 8 + j
                                p.tr(bv[:, j * 128:(j + 1) * 128], ur[:, c * 128:(c + 1) * 128], ident_b.v)
                            p.copy("act", uT[:, g * 8:(g + 1) * 8, :].rearrange("p c j -> p (c j)"), bv)

                    def hidden(i_):
                        uT = uTs[i_ % 2]; gt = gti[i_ % 2]
                        GH = GHs[(i_ // IG) % 2]; il = i_ % IG
                        for tb in range(2):
                            t0 = hf * 1024 + tb * 512
                            bk = bank()
                            for c in range(16):
                                p.mm(bk.v, uT[:, c, :], h1T[:, c, t0:t0 + 512], start=(c == 0), stop=(c == 15))
                            ge = gel[gi[0] % 2]; gi[0] += 1
                            p.act(ge.v, bk.v, AF.Gelu)
                            p.tt("dve", GH[:, il, tb * 512:(tb + 1) * 512], ge.v, gt[:, tb * 512:(tb + 1) * 512],
                                 ALU.mult)

                    def outmm(ig, tt_):
                        GH = GHs[ig % 2]; vs = vss[ig % 2]
                        bks = [bank() for _ in range(4)]
                        for j in range(4):
                            for il in range(IG):
                                p.mm(bks[j].v, GH[:, il, tt_ * 128:(tt_ + 1) * 128], vs[:, il, j * 512:(j + 1) * 512],
                                     start=(il == 0), stop=(il == IG - 1))
                        for j in range(4):
                            a_ = acc[:, tt_, j * 512:(j + 1) * 512]
                            if ig == 0:
                                p.copy("dve", a_, bks[j].v)
                            else:
                                p.tt("dve", a_, a_, bks[j].v, ALU.add)

                    sched = {0: [0, 1, 2], 1: [3, 4, 5], 2: [6, 7], 3: []}
                    load(0)
                    load(1)
                    prep(0)
                    for i_ in range(128):
                        ig = i_ // IG
                        if i_ + 1 < 128:
                            prep(i_ + 1)
                        hidden(i_)
                        if ig >= 1:
                            for k in sched[i_ % IG]:
                                outmm(ig - 1, k)
                        if i_ + 2 < 128:
                            load(i_ + 2)
                    for tt_ in range(8):
                        outmm(128 // IG - 1, tt_)
                with p.scope() as s5_:
                    gB2 = p.sb(s5_, "gB2", [128, D], F32)
                    bB2 = p.sb(s5_, "bB2", [128, D], F32)
                    p.dma("sp", gB2.v, V(ln2_g, ln2_g.h.ap().partition_broadcast(128)))
                    p.dma("sp", bB2.v, V(ln2_b, ln2_b.h.ap().partition_broadcast(128)))
                    h1s = [p.sb(s5_, "h1s%d" % i, [128, D], F32) for i in range(2)]
                    ots = [p.sb(s5_, "ots%d" % i, [128, D], F32) for i in range(2)]
                    junk2s = [p.sb(s5_, "junk2%d" % i, [128, D], BF) for i in range(2)]
                    st2s = [p.sb(s5_, "st2%d" % i, [128, 8], F32) for i in range(2)]
                    for tt_ in range(8):
                        t = hf * 8 + tt_
                        h1 = h1s[tt_ % 2]; ot = ots[tt_ % 2]
                        p.dma("sp", h1.v, h1d[t * 128:(t + 1) * 128, :])
                        p.stt("dve", h1.v, h1.v, ALPHA, acc[:, tt_, :], ALU.mult, ALU.add)
                        layer_norm(128, h1.v, ot.v, gB2.v, bB2.v, st2s[tt_ % 2], junk2s[tt_ % 2].v)
                        p.dma("sp", out_d[t * 128:(t + 1) * 128, :], ot.v)


        p.barrier()
    return nc, dbg


def make_consts():
    ident = np.eye(128, dtype=np.float32)
    tri = np.triu(np.ones((128, 128), dtype=np.float32))
    boh = np.zeros((32, 512), dtype=np.float32)
    rlt = [[15, 165], [14, 27], [13, 18], [12, 14], [11, 9], [10, 7], [9, 4], [8, 4], [7, 1], [6, 1], [5, 1],
           [4, 1], [3, 1], [2, 1], [1, 1], [0, 1], [17, 1], [18, 1], [19, 1], [20, 1], [21, 1], [22, 1], [23, 1],
           [24, 4], [25, 4], [26, 7], [27, 9], [28, 14], [29, 18], [30, 27], [31, 37]]
    bucket = []
    for v, n in rlt:
        bucket += [v] * n
    for i in range(383):
        boh[bucket[i], i] = 1.0
    jrev = np.ascontiguousarray(np.eye(128, dtype=np.float32)[::-1])
    return {"c_ident": ident, "c_tri": tri, "c_boh": boh, "c_jrev": jrev}


def core_inputs(inputs, b):
    m = {
        "x": np.ascontiguousarray(inputs["x"][b]),
        "meta_tokens": np.ascontiguousarray(inputs["meta_tokens"]),
        "ln0_g": inputs["ln0_g"].reshape(1, D), "ln0_b": inputs["ln0_b"].reshape(1, D),
        "rel_bias": np.ascontiguousarray(inputs["rel_bias"]),
        "w_in": np.ascontiguousarray(inputs["w_in"][0]),
        "w_uk": np.ascontiguousarray(inputs["w_uk"][0]), "w_uv": np.ascontiguousarray(inputs["w_uv"][0]),
        "w_gk2": np.ascontiguousarray(inputs["w_gk2"][0]), "b_gk": inputs["b_gk"].reshape(1, 512),
        "gla_norm_g": inputs["gla_norm_g"].reshape(1, 256),
        "w_out": np.ascontiguousarray(inputs["w_out"][0]),
        "ln1_g": inputs["ln1_g"].reshape(1, D), "ln1_b": inputs["ln1_b"].reshape(1, D),
        "w_pq": np.ascontiguousarray(inputs["w_pq"][0]),
        "sub_keys": np.ascontiguousarray(inputs["sub_keys"][0]).reshape(16, 128, 128),
        "u_tab": np.ascontiguousarray(inputs["u_tab"][0]), "v_tab": np.ascontiguousarray(inputs["v_tab"][0]),
        "ln2_g": inputs["ln2_g"].reshape(1, D), "ln2_b": inputs["ln2_b"].reshape(1, D),
    }
    m.update(make_consts())
    return {k: np.asarray(v, dtype=np.float32) for k, v in m.items()}


def kernel(**inputs):
    inputs = {k: np.asarray(v) for k, v in inputs.items()}
    nc, _ = build()
    in_maps = [core_inputs(inputs, b) for b in range(8)]
    res = run_bass_kernel_spmd(nc, in_maps, core_ids=list(range(8)))
    return np.stack([np.asarray(r["out"]) for r in res.results], axis=0).astype(np.float32)
```

```python
import contextlib
import math
import numpy as np
import ml_dtypes
import concourse.bass as bass
import concourse.mybir as mybir
from concourse.bass_utils import run_bass_kernel_spmd

F32 = mybir.dt.float32
BF = mybir.dt.bfloat16
ALU = mybir.AluOpType
AF = mybir.ActivationFunctionType
AX = mybir.AxisListType

D = 2048
S = 2048
NM = 16
L = S + NM
NT = S // 128
EPS = 1e-5
ALPHA = 2.0 ** 0.25
IN_COLS = 5472
O_AQ, O_CKV, O_IQ, O_IK, O_IW, O_GQ, O_GK, O_GV, O_GR, O_GO = (
    0, 1024, 1280, 2304, 2368, 2384, 2896, 3408, 4432, 4448)
NEG = -1.0e30


class Trk:
    def __init__(self, h, name, sb):
        self.h = h
        self.name = name
        self.sb = sb
        self.w = {}
        self.r = {}
        self.dsem = None

    def __getitem__(self, k):
        return V(self, self.h[k])

    @property
    def v(self):
        return V(self, self.h.ap() if hasattr(self.h, "ap") else self.h[:])


class V:
    def __init__(self, o, ap):
        self.o = o
        self.ap = ap

    def __getitem__(self, k):
        return V(self.o, self.ap[k])

    def bitcast(self, dt):
        return V(self.o, self.ap.bitcast(dt))

    def rearrange(self, pattern_, **kw):
        return V(self.o, self.ap.rearrange(pattern_, **kw))

    def bc(self, shape):
        return V(self.o, self.ap.to_broadcast(shape))


class Prog:
    def __init__(self, nc, es):
        self.nc = nc
        self.es = es
        self.e = dict(pe=nc.tensor, act=nc.scalar, dve=nc.vector, pool=nc.gpsimd, sp=nc.sync)
        self.sem = {}
        self.cnt = {}
        for k in ("pe", "act", "dve", "pool"):
            self.sem[k] = es.enter_context(nc.semaphore("S_" + k))
            self.cnt[k] = 0
        self.waited = {k: {} for k in self.e}
        self.dfree = []
        self.dfree_sw = []
        self.ndsem = 0
        self.bank_i = 0
        self.n_inst = 0

    def sb(self, stack, name, shape, dt):
        self.n_sb = getattr(self, "n_sb", 0) + 1
        name = "%s_%d" % (name, self.n_sb)
        h = stack.enter_context(self.nc.sbuf_tensor(name, list(shape), dt))
        t = Trk(h, name, True)
        if hasattr(stack, "tiles"):
            stack.tiles.append(t)
        return t

    def dram(self, name, shape, dt, kind=None):
        if kind:
            h = self.nc.dram_tensor(name, list(shape), dt, kind=kind)
        else:
            h = self.nc.dram_tensor(name, list(shape), dt)
        return Trk(h, name, False)

    def _get_dsem(self, t, sw):
        if t.dsem is None:
            t.dsem = {}
        if sw not in t.dsem:
            free = self.dfree_sw if sw else self.dfree
            if free:
                t.dsem[sw] = free.pop()
            else:
                self.ndsem += 1
                key = ("W%d" if sw else "D%d") % self.ndsem
                self.sem[key] = self.es.enter_context(self.nc.semaphore(key))
                self.cnt[key] = 0
                t.dsem[sw] = key
        return t.dsem[sw]

    @contextlib.contextmanager
    def scope(self):
        st = contextlib.ExitStack()
        st.tiles = []
        try:
            yield st
        finally:
            self.barrier()
            self.release(*st.tiles)
            st.close()

    def release(self, *ts):
        for t in ts:
            if t.dsem is not None:
                for sw, key in t.dsem.items():
                    (self.dfree_sw if sw else self.dfree).append(key)
                t.dsem = None

    def _wait(self, eng, key, val):
        if self.waited[eng].get(key, 0) >= val:
            return
        self.e[eng].wait_ge(self.sem[key], val)
        self.waited[eng][key] = val

    def _sync(self, eng, reads, writes):
        need = {}
        for t in reads:
            for k, v in t.w.items():
                if need.get(k, 0) < v:
                    need[k] = v
        for t in writes:
            for k, v in t.w.items():
                if need.get(k, 0) < v:
                    need[k] = v
            for k, v in t.r.items():
                if need.get(k, 0) < v:
                    need[k] = v
        for k, v in need.items():
            if k == eng and eng == "pe":
                continue
            self._wait(eng, k, v)

    def _done(self, key, val, reads, writes):
        for t in reads:
            t.r[key] = val
        for t in writes:
            t.w = {key: val}
            t.r = {}

    def op(self, eng, reads, writes, fn):
        reads = [x.o for x in reads if isinstance(x, V)]
        writes = [x.o for x in writes]
        self._sync(eng, reads, writes)
        ins = fn(self.e[eng])
        self.cnt[eng] += 1
        ins.then_inc(self.sem[eng], 1)
        self._done(eng, self.cnt[eng], reads, writes)
        self.n_inst += 1

    def dma(self, q, out, in_, **kw):
        sbt = out.o if out.o.sb else in_.o
        self._sync(q, [in_.o], [out.o])
        ins = self.e[q].dma_start(out=out.ap, in_=in_.ap, **kw)
        key = self._get_dsem(sbt, q == "pool")
        self.cnt[key] += 16
        ins.then_inc(self.sem[key], 16)
        self._done(key, self.cnt[key], [in_.o], [out.o])
        self.n_inst += 1

    def barrier(self, engines=("pe", "act", "dve", "pool", "sp")):
        for e in engines:
            for k, v in self.cnt.items():
                if v > 0 and k != e:
                    self._wait(e, k, v)

    def mm(self, out, lhsT, rhs, start=True, stop=True):
        self.op("pe", [lhsT, rhs], [out],
                lambda e: e.matmul(out.ap, lhsT.ap, rhs.ap, start=start, stop=stop))

    def tr(self, out, in_, ident):
        self.op("pe", [in_, ident], [out], lambda e: e.transpose(out.ap, in_.ap, ident.ap))

    def act(self, out, in_, func, bias=0.0, scale=1.0, accum=None, eng="act"):
        rd = [in_, bias, scale]
        wr = [out] + ([accum] if accum is not None else [])
        b = bias.ap if isinstance(bias, V) else bias
        sc = scale.ap if isinstance(scale, V) else scale
        kw = {}
        if accum is not None:
            kw["accum_out"] = accum.ap
        self.op("act", rd, wr, lambda e: e.activation(out.ap, in_.ap, func, bias=b, scale=sc, **kw))

    def tt(self, eng, out, in0, in1, op):
        self.op(eng, [in0, in1], [out], lambda e: e.tensor_tensor(out.ap, in0.ap, in1.ap, op))

    def ts(self, eng, out, in0, s1, s2, op0, op1=None, accum=None):
        a1 = s1.ap if isinstance(s1, V) else s1
        a2 = s2.ap if isinstance(s2, V) else s2
        kw = {}
        if op1 is not None:
            kw["op1"] = op1
        if accum is not None:
            kw["accum_out"] = accum.ap
        wr = [out] + ([accum] if accum is not None else [])
        self.op(eng, [in0, s1, s2], wr,
                lambda e: e.tensor_scalar(out.ap, in0.ap, a1, a2, op0, **kw))

    def stt(self, eng, out, in0, sc, in1, op0, op1):
        a = sc.ap if isinstance(sc, V) else sc
        self.op(eng, [in0, sc, in1], [out],
                lambda e: e.scalar_tensor_tensor(out.ap, in0.ap, a, in1.ap, op0, op1))

    def copy(self, eng, out, in_):
        if eng == "act":
            self.op(eng, [in_], [out], lambda e: e.copy(out.ap, in_.ap))
        else:
            self.op(eng, [in_], [out], lambda e: e.tensor_copy(out.ap, in_.ap))

    def memset(self, eng, out, val):
        self.op(eng, [], [out], lambda e: e.memset(out.ap, val))

    def rsum(self, eng, out, in_):
        self.op(eng, [in_], [out], lambda e: e.reduce_sum(out.ap, in_.ap, AX.X))

    def recip(self, out, in_):
        self.op("dve", [in_], [out], lambda e: e.reciprocal(out.ap, in_.ap))

    def max8(self, out, in_):
        self.op("dve", [in_], [out], lambda e: e.max(out=out.ap, in_=in_.ap))

    def mrep(self, out, rep, vals, imm):
        self.op("dve", [rep, vals], [out],
                lambda e: e.match_replace(out=out.ap, in_to_replace=rep.ap, in_values=vals.ap,
                                          imm_value=imm))


def build(debug=None, stop_after=None):
    nc = bass.Bass("TRN2", target_bir_lowering=False)
    es = contextlib.ExitStack()
    with es:
        p = Prog(nc, es)
        IN = {}

        def inp(name, shape, dt=F32):
            IN[name] = p.dram(name, shape, dt, kind="ExternalInput")
            return IN[name]

        x = inp("x", [S, D])
        meta = inp("meta_tokens", [NM, D])
        ln0_g = inp("ln0_g", [1, D]); ln0_b = inp("ln0_b", [1, D])
        rel_bias = inp("rel_bias", [32, 8])
        w_in = inp("w_in", [D, IN_COLS])
        w_uk = inp("w_uk", [8, 256, 128]); w_uv = inp("w_uv", [8, 256, 128])
        w_gk2 = inp("w_gk2", [16, 512]); b_gk = inp("b_gk", [1, 512])
        gla_g = inp("gla_norm_g", [1, 256])
        w_out = inp("w_out", [D, D])
        ln1_g = inp("ln1_g", [1, D]); ln1_b = inp("ln1_b", [1, D])
        w_pq = inp("w_pq", [D, D])
        sub_keys = inp("sub_keys", [16, 128, 128])
        u_tab = inp("u_tab", [16384, D]); v_tab = inp("v_tab", [16384, D])
        ln2_g = inp("ln2_g", [1, D]); ln2_b = inp("ln2_b", [1, D])
        ident_in = inp("c_ident", [128, 128])
        tri_in = inp("c_tri", [128, 128])
        boh_in = inp("c_boh", [32, 512])
        jrev_in = inp("c_jrev", [128, 128])
        out_d = p.dram("out", [S, D], F32, kind="ExternalOutput")

        dbg = {}

        def scratch(name, shape, dt):
            kind = "ExternalOutput" if (debug and name in debug) else None
            t = p.dram(name, shape, dt, kind=kind)
            dbg[name] = t
            return t

        blk = es.enter_context(nc.Block())
        gs = contextlib.ExitStack()
        es.enter_context(gs)
        banks = []
        for i in range(8):
            h = es.enter_context(nc.psum_tensor("bank%d" % i, [128, 512], F32))
            banks.append(Trk(h, "bank%d" % i, True))

        def bank():
            b = banks[p.bank_i % 8]
            p.bank_i += 1
            return b

        ident_f = p.sb(gs, "ident_f", [128, 128], F32)
        ident_b = p.sb(gs, "ident_b", [128, 128], BF)
        tri_f = p.sb(gs, "tri_f", [128, 128], F32)
        tri_b = p.sb(gs, "tri_b", [128, 128], BF)
        p.dma("sp", ident_f.v, ident_in.v)
        p.dma("sp", tri_f.v, tri_in.v)
        p.copy("dve", ident_b.v, ident_f.v)
        p.copy("dve", tri_b.v, tri_f.v)
        jrev_b = p.sb(gs, "jrev_b", [128, 128], BF)
        p.dma("pool", jrev_b.v, jrev_in.v)

        def layer_norm(n, src, dst, gB, bB, st, junk):
            p.rsum("dve", st[:n, 0:1], src)
            p.ts("dve", st[:n, 1:2], st[:n, 0:1], -1.0 / D, None, ALU.mult)
            p.act(src, src, AF.Identity, bias=st[:n, 1:2])
            p.memset("dve", st[:n, 2:3], 0.0)
            p.act(junk, src, AF.Square, accum=st[:n, 2:3])
            p.ts("dve", st[:n, 3:4], st[:n, 2:3], 1.0 / D, EPS, ALU.mult, ALU.add)
            p.act(st[:n, 5:6], st[:n, 3:4], AF.Sqrt)
            p.recip(st[:n, 4:5], st[:n, 5:6])
            p.stt("dve", src, src, st[:n, 4:5], gB, ALU.mult, ALU.mult)
            p.tt("dve", src, src, bB, ALU.add)
            p.copy("act", dst, src)

        FT = {}
        for nm, rows in (("qlT", 2048), ("ckvT", 256), ("iqT", 1024), ("ikT", 128),
                         ("gqT", 512), ("gkT", 512), ("grT", 16)):
            FT[nm] = scratch(nm, [rows, L], BF)
        TM = {}
        TM["ckv"] = scratch("ckv", [L, 256], BF)
        TM["iw"] = scratch("iw", [L, 16], F32)
        TM["gv"] = scratch("gv", [L, 1024], BF)
        TM["go"] = scratch("go", [L, 1024], BF)
        hd = scratch("hd", [S, D], F32)
        h1d = scratch("h1d", [S, D], F32)

        sA = contextlib.ExitStack()
        sA.tiles = []
        hTm = p.sb(sA, "hTm", [128, 16, NM], BF)
        hTb = [p.sb(sA, "hTb%d" % i, [128, 16, 512], BF) for i in range(4)]

        def hT_cols(c0, n):
            if c0 < NM:
                assert c0 + n <= NM
                return hTm, slice(c0, c0 + n)
            b = (c0 - NM) // 512
            o = (c0 - NM) % 512
            assert o + n <= 512
            return hTb[b], slice(o, o + n)
        if True:
            s0 = sA
            gB = p.sb(s0, "gB", [128, D], F32)
            bB = p.sb(s0, "bB", [128, D], F32)
            p.dma("sp", gB.v, V(ln0_g, ln0_g.h.ap().partition_broadcast(128)))
            p.dma("sp", bB.v, V(ln0_b, ln0_b.h.ap().partition_broadcast(128)))
            xts = [p.sb(s0, "xt%d" % i, [128, D], F32) for i in range(2)]
            zts = [p.sb(s0, "zt%d" % i, [128, D], BF) for i in range(2)]
            junks = [p.sb(s0, "junk%d" % i, [128, D], BF) for i in range(2)]
            sts0 = [p.sb(s0, "st%d" % i, [128, 8], F32) for i in range(2)]
            for ti in range(NT + 1):
                n = NM if ti == 0 else 128
                c0 = 0 if ti == 0 else NM + (ti - 1) * 128
                xt = xts[ti % 2]; zt = zts[ti % 2]
                if ti == 0:
                    p.dma("sp", xt[:n, :], meta.v)
                else:
                    p.dma("sp", xt[:n, :], x[(ti - 1) * 128: ti * 128, :])
                layer_norm(n, xt[:n, :], zt[:n, :], gB[:n, :], bB[:n, :], sts0[ti % 2], junks[ti % 2][:n, :])
                if ti > 0:
                    p.dma("act", hd[(ti - 1) * 128: ti * 128, :], xt.v)
                for half in range(2):
                    bk = bank()
                    bv = bk.v.bitcast(BF)
                    for c in range(8):
                        cc = half * 8 + c
                        p.tr(bv[:, c * 128: c * 128 + n], zt[:n, cc * 128:(cc + 1) * 128],
                             ident_b[:n, :n])
                    src = bv.rearrange("p (c t) -> p c t", t=128)[:, :, :n]
                    ht_, sl_ = hT_cols(c0, n)
                    p.copy("act", ht_[:, half * 8:(half + 1) * 8, sl_], src)
        with p.scope() as s1:
            wsl = [p.sb(s1, "wsl%d" % i, [128, 16, 128], BF) for i in range(3)]
            stg = [p.sb(s1, "stg%d" % i, [128, 512], BF) for i in range(4)]
            stgf = [p.sb(s1, "stgf%d" % i, [128, 16], F32) for i in range(2)]
            wukT = p.sb(s1, "wukT", [128, 8, 256], BF)
            w_in_r = w_in.h.ap().rearrange("(c p) n -> p c n", p=128)
            with p.scope() as s2:
                wuk_n = p.sb(s2, "wuk_n", [128, 16, 128], BF)
                p.dma("pool", wuk_n.v, V(w_uk, w_uk.h.ap().rearrange("h (rc p) d -> p (h rc) d", p=128)))
                for g in range(2):
                    bk = bank(); bv = bk.v.bitcast(BF)
                    for j in range(8):
                        hr = g * 8 + j
                        p.tr(bv[:, j * 128:(j + 1) * 128], wuk_n[:, hr, :], ident_b.v)
                    p.copy("dve", wukT[:, g * 4:(g + 1) * 4, :].rearrange("p h r -> p (h r)"), bv)
            wi = [0]
            si = [0]

            def load_w(col0, ncols, dup=False):
                w = wsl[wi[0] % 3]; wi[0] += 1
                if dup:
                    p.dma("pool", w[:, :, 0:ncols], V(w_in, w_in_r[:, :, col0:col0 + ncols]))
                    p.dma("pool", w[:, :, ncols:2 * ncols], V(w_in, w_in_r[:, :, col0:col0 + ncols]))
                else:
                    p.dma("pool", w[:, :, 0:ncols], V(w_in, w_in_r[:, :, col0:col0 + ncols]))
                return w

            tok_blocks = [(NM + i * 512, 512) for i in range(4)] + [(0, NM)]

            def fm_group(col0, rows, dst, dst_row0, real_only=False, dup=False, post=None):
                w = load_w(col0, rows // 2 if dup else rows, dup)
                for (t0, tn) in tok_blocks:
                    if real_only and t0 == 0:
                        continue
                    bk = bank()
                    ht_, sl_ = hT_cols(t0, tn)
                    for c in range(16):
                        p.mm(bk[:rows, :tn], w[:, c, :rows], ht_[:, c, sl_],
                             start=(c == 0), stop=(c == 15))
                    sg = stg[si[0] % 4]; si[0] += 1
                    if si[0] % 2:
                        p.copy("act", sg[:rows, :tn], bk[:rows, :tn])
                    else:
                        p.copy("dve", sg[:rows, :tn], bk[:rows, :tn])
                    if post is not None:
                        post(sg, t0, tn)
                    else:
                        p.dma("sp", dst[dst_row0:dst_row0 + rows, t0:t0 + tn], sg[:rows, :tn])

            for h in range(8):
                def post(sg, t0, tn, h=h):
                    for rc in range(2):
                        bk = bank()
                        p.mm(bk[:, :tn], wukT[:, h, rc * 128:(rc + 1) * 128], sg[:, :tn])
                        s2_ = stg[si[0] % 4]; si[0] += 1
                        p.act(s2_[:, :tn], bk[:, :tn], AF.Copy, scale=128.0 ** -0.5)
                        r0 = (h * 2 + rc) * 128
                        p.dma("sp", FT["qlT"][r0:r0 + 128, t0:t0 + tn], s2_[:, :tn])
                fm_group(O_AQ + h * 128, 128, None, 0, real_only=True, post=post)
            for c in range(2):
                fm_group(O_CKV + c * 128, 128, FT["ckvT"], c * 128)
            for c in range(8):
                fm_group(O_IQ + c * 128, 128, FT["iqT"], c * 128, real_only=True)
            fm_group(O_IK, 128, FT["ikT"], 0, dup=True)
            for c in range(4):
                fm_group(O_GQ + c * 128, 128, FT["gqT"], c * 128, real_only=True)
            for c in range(4):
                fm_group(O_GK + c * 128, 128, FT["gkT"], c * 128)
            fm_group(O_GR, 16, FT["grT"], 0)

            with p.scope() as s3:
                wtm = [p.sb(s3, "wtm%d" % i, [128, 16, 512], BF) for i in range(2)]
                k = 0
                for (col0, ncols, dst, dcol0, real_only) in (
                        (O_CKV, 256, TM["ckv"], 0, False),
                        (O_IW, 16, TM["iw"], 0, True),
                        (O_GV, 512, TM["gv"], 0, False), (O_GV + 512, 512, TM["gv"], 512, False),
                        (O_GO, 512, TM["go"], 0, True), (O_GO + 512, 512, TM["go"], 512, True)):
                    w = wtm[k % 2]; k += 1
                    p.dma("pool", w[:, :, :ncols], V(w_in, w_in_r[:, :, col0:col0 + ncols]))
                    for ti in range(NT + 1):
                        if real_only and ti == 0:
                            continue
                        n = NM if ti == 0 else 128
                        c0 = 0 if ti == 0 else NM + (ti - 1) * 128
                        bk = bank()
                        ht_, sl_ = hT_cols(c0, n)
                        for c in range(16):
                            p.mm(bk[:n, :ncols], ht_[:, c, sl_], w[:, c, :ncols],
                                 start=(c == 0), stop=(c == 15))
                        if dst is TM["iw"]:
                            sg = stgf[si[0] % 2]; si[0] += 1
                            p.copy("dve", sg[:n, :ncols], bk[:n, :ncols])
                        else:
                            sg = stg[si[0] % 4]; si[0] += 1
                            if si[0] % 2:
                                p.copy("act", sg[:n, :ncols], bk[:n, :ncols])
                            else:
                                p.copy("dve", sg[:n, :ncols], bk[:n, :ncols])
                        p.dma("sp", dst[c0:c0 + n, dcol0:dcol0 + ncols], sg[:n, :ncols])
        p.barrier()
        p.release(*sA.tiles)
        sA.close()

        if stop_after == "B":
            p.barrier()
            return nc, dbg

        def bank6():
            b = banks[p.bank_i % 6]
            p.bank_i += 1
            return b
        ob_i = [0]

        def obank():
            ob_i[0] += 1
            return banks[6 + ob_i[0] % 2]

        sY = contextlib.ExitStack()
        sY.tiles = []
        es.enter_context(sY)
        yT = p.sb(sY, "yT", [128, 16, S], BF)
        yTd = scratch("yTd", [128, 16 * S], BF)

        Etab = scratch("Etab", [8, 512], F32)
        if debug and "scoreD" in debug:
            scratch("scoreD", [128, L], F32)
            scratch("thrD", [128, 1], F32)
            scratch("olD", [128, 2048], BF)
            scratch("maskTD", [128, 17 * 128], BF)
        with p.scope() as sc:
            ikT = p.sb(sc, "ikT_s", [128, L], BF)
            ckvT = p.sb(sc, "ckvT_s", [128, 2, L], BF)
            ckva = p.sb(sc, "ckva", [128, 17, 257], BF)
            wuv = p.sb(sc, "wuv", [128, 16, 128], BF)
            BT = p.sb(sc, "BT", [128, 8, 4, 128], BF)
            p.dma("sp", ikT.v, FT["ikT"].v)
            p.dma("sp", ckvT.v, V(FT["ckvT"], FT["ckvT"].h.ap().rearrange("(c p) t -> p c t", p=128)))
            p.memset("dve", ckva.v, 1.0)
            p.dma("sp", ckva[:NM, 0, 0:256], TM["ckv"][0:NM, :])
            p.dma("sp", ckva[:, 1:17, 0:256],
                  V(TM["ckv"], TM["ckv"].h[NM:L, :].rearrange("(t p) r -> p t r", p=128)))
            p.dma("pool", wuv.v, V(w_uv, w_uv.h.ap().rearrange("h (rc p) d -> p (h rc) d", p=128)))
            with p.scope() as sb_:
                rb = p.sb(sb_, "rb", [32, 8], F32)
                boh = p.sb(sb_, "boh", [32, 512], F32)
                es_ = p.sb(sb_, "es_", [8, 512], F32)
                p.dma("sp", rb.v, rel_bias.v)
                p.dma("sp", boh.v, boh_in.v)
                bk = bank6()
                p.mm(bk[:8, :], rb.v, boh.v)
                p.copy("dve", es_.v, bk[:8, :])
                p.dma("sp", Etab.v, es_.v)
            for h in range(8):
                for kind, (off, ncol, ps) in enumerate(((128, 128, 1), (0, 128, 1), (0, 128, 0), (112, NM, 1))):
                    src = bass.AP(tensor=Etab.h, offset=h * 512 + off, ap=[[ps, 128], [1, ncol]])
                    p.dma("pool", BT[:, h, kind, :ncol], V(Etab, src))

            iqs = [p.sb(sc, "iqs%d" % i, [128, 8, 128], BF) for i in range(2)]
            qls = [p.sb(sc, "qls%d" % i, [128, 16, 128], BF) for i in range(3)]
            iws = [p.sb(sc, "iws%d" % i, [128, 16], F32) for i in range(2)]
            scores_ = [p.sb(sc, "score%d" % i, [128, L], F32) for i in range(2)]
            rl = [p.sb(sc, "rl%d" % i, [128, 512], F32) for i in range(4)]
            NB = 16
            ck = p.sb(sc, "ck", [128, NB], F32)
            for k in range(NB):
                p.memset("pool", ck[:, k:k + 1], 0.5 ** (k + 1))
            bsts = [p.sb(sc, "bst%d" % i, [128, 8], F32) for i in range(2)]
            nRks = [p.sb(sc, "nRk%d" % i, [128, NB], F32) for i in range(2)]
            cnts = [p.sb(sc, "cnt%d" % i, [128, NB], F32) for i in range(2)]
            sjunk = p.sb(sc, "sjunk", [128, L], BF)
            thr = p.sb(sc, "thr", [128, 1], F32)
            maskq = p.sb(sc, "maskq", [128, L], BF)
            maskTs = [p.sb(sc, "maskT%d" % i, [128, 17, 128], BF) for i in range(2)]
            PTs = [p.sb(sc, "PT%d" % i, [128, 512], BF) for i in range(4)]
            rden = p.sb(sc, "rden", [128, 16], F32)
            olsb = p.sb(sc, "olsb", [128, 8, 256], BF)
            olT = p.sb(sc, "olT", [128, 16, 128], BF)
            iqT_r = FT["iqT"].h.ap().rearrange("(c p) t -> p c t", p=128)
            qlT_r = FT["qlT"].h.ap().rearrange("(c p) t -> p c t", p=128)
            rli = [0]; pti = [0]

            def tiles_of(tq):
                return [(0, 0, NM)] + [(i + 1, NM + 128 * i, 128) for i in range(tq + 1)]

            def bisect_steps(tq):
                Wk = NM + 128 * (tq + 1)
                if Wk <= 256:
                    return
                score = scores_[tq % 2]; bst = bsts[tq % 2]; nRk = nRks[tq % 2]; cnt = cnts[tq % 2]
                for k in range(NB):
                    p.ts("dve", sjunk[:, :Wk], score[:, :Wk], bst[:, 4:5], 0.0, ALU.is_ge, ALU.add,
                         accum=cnt[:, k:k + 1])
                    p.stt("dve", bst[:, 5:6], cnt[:, k:k + 1], 256.0, nRk[:, k:k + 1], ALU.is_ge, ALU.mult)
                    kn_ = k + 1 if k + 1 < NB else k
                    p.stt("dve", bst[:, 4:5], bst[:, 4:5], nRk[:, kn_:kn_ + 1], bst[:, 5:6], ALU.subtract, ALU.add)
                    yield

            def scores(tq, steps):
                q0 = NM + tq * 128
                Wk = NM + 128 * (tq + 1)
                iq = iqs[tq % 2]; ql = qls[tq % 3]; iw = iws[tq % 2]
                score = scores_[tq % 2]; bst = bsts[tq % 2]; nRk = nRks[tq % 2]; cnt = cnts[tq % 2]
                p.dma("sp", iq.v, V(FT["iqT"], iqT_r[:, :, q0:q0 + 128]))
                p.dma("sp", ql.v, V(FT["qlT"], qlT_r[:, :, q0:q0 + 128]))
                p.dma("sp", iw.v, TM["iw"][q0:q0 + 128, :])
                nblk = len(range(0, Wk, 512))
                for bi, k0 in enumerate(range(0, Wk, 512)):
                    kn = min(512, Wk - k0)
                    for h in range(16):
                        bk = bank6()
                        pb = (h % 2) * 64
                        p.mm(bk[:, :kn], iq[pb:pb + 64, h // 2, :], ikT[pb:pb + 64, k0:k0 + kn])
                        r = rl[rli[0] % 4]; rli[0] += 1
                        p.act(r[:, :kn], bk[:, :kn], AF.Relu)
                        if h == 0:
                            p.ts("dve", score[:, k0:k0 + kn], r[:, :kn], iw[:, 0:1], None, ALU.mult)
                        else:
                            p.stt("dve", score[:, k0:k0 + kn], r[:, :kn], iw[:, h:h + 1],
                                  score[:, k0:k0 + kn], ALU.mult, ALU.add)
                        if h % 4 == 3 and steps is not None:
                            next(steps, None)
                if steps is not None:
                    for _ in steps:
                        pass
                if Wk > 256:
                    p.op("dve", [score.v], [bst.v],
                         lambda e: e.tensor_reduce(bst[:, 0:1].ap, score[:, :Wk].ap, AX.X, ALU.max))
                    p.op("dve", [score.v], [bst.v],
                         lambda e: e.tensor_reduce(bst[:, 1:2].ap, score[:, :Wk].ap, AX.X, ALU.min))
                    p.tt("dve", bst[:, 2:3], bst[:, 0:1], bst[:, 1:2], ALU.subtract)
                    p.ts("dve", bst[:, 2:3], bst[:, 2:3], 2.0, None, ALU.add)
                    p.ts("dve", nRk.v, ck.v, bst[:, 2:3], None, ALU.mult)
                    p.stt("dve", bst[:, 4:5], bst[:, 1:2], -1.0, nRk[:, 0:1], ALU.add, ALU.add)
                p.memset("dve", score[0:64, Wk - 64:Wk], NEG)

            def finalize(tq):
                Wk = NM + 128 * (tq + 1)
                score = scores_[tq % 2]; bst = bsts[tq % 2]
                if Wk > 256:
                    p.ts("dve", thr.v, bst[:, 4:5], -1.0e29, None, ALU.max)
                else:
                    p.memset("dve", thr.v, -1.0e29)
                p.ts("dve", maskq[:, :Wk], score[:, :Wk], thr[:, 0:1], None, ALU.is_ge)

            def mask_transposes(tq):
                maskT = maskTs[tq % 2]
                tiles = tiles_of(tq)
                for g0 in range(0, len(tiles), 8):
                    grp = tiles[g0:g0 + 8]
                    bk = bank6(); bv = bk.v.bitcast(BF)
                    for j, (ti, c0, kn) in enumerate(grp):
                        p.tr(bv[:kn, j * 128:(j + 1) * 128], maskq[:, c0:c0 + kn], ident_b.v)
                    if grp[0][0] == 0:
                        p.copy("act", maskT[:NM, 0, :], bv[:NM, 0:128])
                        if len(grp) > 1:
                            p.copy("act", maskT[:, 1:len(grp), :].rearrange("p t q -> p (t q)"),
                                   bv[:, 128:128 * len(grp)])
                    else:
                        p.copy("act", maskT[:, grp[0][0]:grp[0][0] + len(grp), :].rearrange("p t q -> p (t q)"),
                               bv[:, 0:128 * len(grp)])

            def attention(tq):
                maskT = maskTs[tq % 2]
                ql = qls[tq % 3]
                tiles = tiles_of(tq)
                pend = []

                def flush(item):
                    h, ob, PT, grp, g0, first_ = item
                    for j, (ti, c0, kn) in enumerate(grp):
                        last = (g0 + j == len(tiles) - 1)
                        p.mm(ob[:, 0:257], PT[:kn, j * 128:(j + 1) * 128], ckva[:kn, ti, :],
                             start=(first_ and j == 0), stop=last)
                    if g0 + len(grp) == len(tiles):
                        p.act(rden[:, 8 + h:9 + h], ob[:, 256:257], AF.Ln)
                        p.act(rden[:, h:h + 1], rden[:, 8 + h:9 + h], AF.Exp, scale=-1.0)
                        p.act(olsb[:, h, :], ob[:, 0:256], AF.Copy, scale=rden[:, h:h + 1])

                for h in range(8):
                    ob = obank()
                    for g0 in range(0, len(tiles), 4):
                        grp = tiles[g0:g0 + 4]
                        bk = bank6()
                        for j, (ti, c0, kn) in enumerate(grp):
                            if ti == 0:
                                kind = 3 if tq == 0 else 2
                            else:
                                dlt = tq - (ti - 1)
                                kind = 0 if dlt == 0 else (1 if dlt == 1 else 2)
                            o_ = bk[:kn, j * 128:(j + 1) * 128]
                            p.mm(o_, ckvT[:, 0, c0:c0 + kn], ql[:, h * 2, :], start=True, stop=False)
                            p.mm(o_, ckvT[:, 1, c0:c0 + kn], ql[:, h * 2 + 1, :], start=False, stop=False)
                            p.mm(o_, BT[:, h, kind, :kn], jrev_b.v, start=False, stop=True)
                        PT = PTs[pti[0] % 4]; pti[0] += 1
                        w_ = 128 * len(grp)
                        if grp[0][0] == 0:
                            p.act(PT[:NM, 0:128], bk[:NM, 0:128], AF.Exp)
                            p.tt("pool", PT[:NM, 0:128], PT[:NM, 0:128], maskT[:NM, 0, :], ALU.mult)
                            if len(grp) > 1:
                                p.act(PT[:, 128:w_], bk[:, 128:w_], AF.Exp)
                                p.tt("pool", PT[:, 128:w_], PT[:, 128:w_],
                                     maskT[:, 1:len(grp), :].rearrange("p t q -> p (t q)"), ALU.mult)
                        else:
                            p.act(PT[:, 0:w_], bk[:, 0:w_], AF.Exp)
                            p.tt("pool", PT[:, 0:w_], PT[:, 0:w_],
                                 maskT[:, grp[0][0]:grp[0][0] + len(grp), :].rearrange("p t q -> p (t q)"),
                                 ALU.mult)
                        pend.append((h, ob, PT, grp, g0, g0 == 0))
                        if len(pend) > 2:
                            flush(pend.pop(0))
                while pend:
                    flush(pend.pop(0))
                for g in range(2):
                    bk = bank6(); bv = bk.v.bitcast(BF)
                    for j in range(8):
                        hr = g * 8 + j
                        p.tr(bv[:, j * 128:(j + 1) * 128], olsb[:, hr // 2, (hr % 2) * 128:(hr % 2 + 1) * 128],
                             ident_b.v)
                    p.copy("act", olT[:, g * 8:(g + 1) * 8, :].rearrange("p c q -> p (c q)"), bv)
                for g in range(2):
                    bk = bank6()
                    for j in range(4):
                        h = g * 4 + j
                        for rc in range(2):
                            p.mm(bk[:, j * 128:(j + 1) * 128], wuv[:, h * 2 + rc, :], olT[:, h * 2 + rc, :],
                                 start=(rc == 0), stop=(rc == 1))
                    p.copy("act", yT[:, g * 4:(g + 1) * 4, tq * 128:(tq + 1) * 128],
                           bk.v.rearrange("p (c q) -> p c q", q=128))

            for it_ in range(NT + 2):
                steps = bisect_steps(it_ - 1) if 1 <= it_ <= NT else None
                if it_ < NT:
                    scores(it_, steps)
                elif steps is not None:
                    for _ in steps:
                        pass
                if 1 <= it_ <= NT:
                    finalize(it_ - 1)
                if it_ >= 2:
                    attention(it_ - 2)
                if 1 <= it_ <= NT:
                    mask_transposes(it_ - 1)
        if stop_after == "C1":
            p.dma("sp", yTd.v, yT.v.rearrange("p c t -> p (c t)"))
            p.barrier()
            return nc, dbg

        sW = contextlib.ExitStack()
        sW.tiles = []
        es.enter_context(sW)
        wout = p.sb(sW, "wout", [128, 16, D], BF)
        w_out_r = w_out.h.ap().rearrange("(c p) n -> p c n", p=128)
        for j in range(4):
            p.dma("pool", wout[:, :, j * 512:(j + 1) * 512], V(w_out, w_out_r[:, :, j * 512:(j + 1) * 512]))
        gB1 = p.sb(sW, "gB1", [128, D], F32)
        bB1 = p.sb(sW, "bB1", [128, D], F32)
        p.dma("sp", gB1.v, V(ln1_g, ln1_g.h.ap().partition_broadcast(128)))
        p.dma("sp", bB1.v, V(ln1_b, ln1_b.h.ap().partition_broadcast(128)))
        with p.scope() as sg_:
            S_f = p.sb(sg_, "S_f", [128, 4, 256], F32)
            S_b = p.sb(sg_, "S_b", [128, 4, 256], BF)
            wgk = p.sb(sg_, "wgk", [16, 512], BF)
            bgk = p.sb(sg_, "bgk", [1, 512], BF)
            ones1 = p.sb(sg_, "ones1", [1, 128], BF)
            gng4 = p.sb(sg_, "gng4", [128, 4, 256], F32)
            p.memset("dve", S_f.v, 0.0)
            p.memset("dve", S_b.v, 0.0)
            p.memset("dve", ones1.v, 1.0)
            p.dma("pool", wgk.v, w_gk2.v)
            p.dma("pool", bgk.v, b_gk.v)
            for hh in range(4):
                p.dma("sp", gng4[:, hh, :], V(gla_g, gla_g.h.ap().partition_broadcast(128)))
            grs = [p.sb(sg_, "grs%d" % i, [16, 128], BF) for i in range(2)]
            gqs = [p.sb(sg_, "gqs%d" % i, [128, 4, 128], BF) for i in range(2)]
            gks = [p.sb(sg_, "gks%d" % i, [128, 4, 128], BF) for i in range(2)]
            vts = [p.sb(sg_, "vts%d" % i, [128, 1024], BF) for i in range(2)]
            gos = [p.sb(sg_, "gos%d" % i, [128, 1024], BF) for i in range(2)]
            lpe = p.sb(sg_, "lpe", [128, 512], F32)
            lp = p.sb(sg_, "lp", [128, 512], F32)
            E1 = p.sb(sg_, "E1", [128, 4, 128], F32)
            E2 = p.sb(sg_, "E2", [128, 4, 128], F32)
            qtl = p.sb(sg_, "qtl", [128, 4, 128], BF)
            ktl = p.sb(sg_, "ktl", [128, 4, 128], BF)
            ktok = p.sb(sg_, "ktok", [128, 4, 128], BF)
            ATs = p.sb(sg_, "ATs", [128, 4, 128], BF)
            tmpS = p.sb(sg_, "tmpS", [128, 256], F32)
            ssq = p.sb(sg_, "ssq", [128, 8], F32)
            gsil = p.sb(sg_, "gsil", [128, 4, 256], F32)
            ybt = p.sb(sg_, "ybt", [128, 1024], BF)
            junkg = p.sb(sg_, "junkg", [128, 256], BF)
            gqT_r = FT["gqT"].h.ap().rearrange("(c p) t -> p c t", p=128)
            gkT_r = FT["gkT"].h.ap().rearrange("(c p) t -> p c t", p=128)
            for ti in range(NT + 1):
                n = NM if ti == 0 else 128
                c0 = 0 if ti == 0 else NM + (ti - 1) * 128
                real = ti > 0
                gr = grs[ti % 2]; gq = gqs[ti % 2]; gk_ = gks[ti % 2]; vt = vts[ti % 2]; go = gos[ti % 2]
                p.dma("sp", gr[:, :n], FT["grT"][:, c0:c0 + n])
                p.dma("sp", gk_[:, :, :n], V(FT["gkT"], gkT_r[:, :, c0:c0 + n]))
                p.dma("sp", vt[:n, :], TM["gv"][c0:c0 + n, :])
                if real:
                    p.dma("sp", gq[:, :, :n], V(FT["gqT"], gqT_r[:, :, c0:c0 + n]))
                    p.dma("sp", go[:n, :], TM["go"][c0:c0 + n, :])
                bA = bank6()
                p.mm(bA[:n, :], gr[:, :n], wgk.v, start=True, stop=False)
                p.mm(bA[:n, :], ones1[:, :n], bgk.v, start=False, stop=True)
                p.act(lpe[:n, :], bA[:n, :], AF.Exp, scale=-1.0)
                p.act(lp[:n, :], lpe[:n, :], AF.Ln, bias=1.0)
                bB_ = bank6()
                for hh in range(4):
                    p.mm(bB_[:, hh * 128: hh * 128 + n], lp[:n, hh * 128:(hh + 1) * 128], tri_f[:n, :n])
                bBv = bB_.v.rearrange("p (h i) -> p h i", i=128)[:, :, :n]
                p.act(E1[:, :, :n], bBv, AF.Exp, scale=-1.0 / 16.0)
                p.act(E2[:, :, :n], bBv, AF.Exp, scale=1.0 / 16.0)
                p.tt("dve", ktl[:, :, :n], gk_[:, :, :n], E2[:, :, :n], ALU.mult)
                bT_ = bank6(); bTv = bT_.v.bitcast(BF)
                for hh in range(4):
                    p.tr(bTv[:n, hh * 128:(hh + 1) * 128], ktl[:, hh, :n], ident_b.v)
                p.copy("act", ktok[:n, :, :].rearrange("p h d -> p (h d)"), bTv[:n, 0:512])
                if real:
                    p.stt("dve", qtl[:, :, :n], gq[:, :, :n], 128.0 ** -0.5, E1[:, :, :n], ALU.mult, ALU.mult)
                    bC = bank6()
                    for hh in range(4):
                        p.mm(bC[:n, hh * 128: hh * 128 + n], ktl[:, hh, :n], qtl[:, hh, :n])
                    for hh in range(4):
                        p.tt("dve", ATs[:n, hh, :n], bC[:n, hh * 128: hh * 128 + n], tri_b[:n, :n], ALU.mult)
                    bO = [bank6(), bank6()]
                    for hh in range(4):
                        o_ = bO[hh // 2][:, (hh % 2) * 256:(hh % 2 + 1) * 256]
                        p.mm(o_, qtl[:, hh, :n], S_b[:, hh, :], start=True, stop=False)
                        p.mm(o_, ATs[:n, hh, :n], vt[:n, hh * 256:(hh + 1) * 256], start=False, stop=True)
                for hh in range(4):
                    bS = bank6()
                    p.mm(bS[:, 0:256], ktok[:n, hh, :], vt[:n, hh * 256:(hh + 1) * 256])
                    p.ts("dve", tmpS.v, bS[:, 0:256], E1[:, hh, n - 1:n], None, ALU.mult)
                    p.stt("dve", S_f[:, hh, :], S_f[:, hh, :], E1[:, hh, n - 1:n], tmpS.v, ALU.mult, ALU.add)
                p.copy("act", S_b.v, S_f.v)
                if real:
                    p.memset("dve", ssq[:, 0:4], 0.0)
                    for hh in range(4):
                        o_ = bO[hh // 2][:, (hh % 2) * 256:(hh % 2 + 1) * 256]
                        p.act(junkg.v, o_, AF.Square, accum=ssq[:, hh:hh + 1])
                    p.ts("dve", ssq[:, 4:8], ssq[:, 0:4], 1.0 / 256.0, EPS, ALU.mult, ALU.add)
                    p.act(ssq[:, 4:8], ssq[:, 4:8], AF.Sqrt)
                    p.recip(ssq[:, 4:8], ssq[:, 4:8])
                    p.act(gsil.v.rearrange("p h v -> p (h v)"), go.v, AF.Silu)
                    p.tt("dve", gsil.v, gsil.v, gng4.v, ALU.mult)
                    for hh in range(4):
                        o_ = bO[hh // 2][:, (hh % 2) * 256:(hh % 2 + 1) * 256]
                        p.stt("dve", ybt[:, hh * 256:(hh + 1) * 256], o_, ssq[:, 4 + hh:5 + hh], gsil[:, hh, :],
                              ALU.mult, ALU.mult)
                    bY = bank6(); bYv = bY.v.bitcast(BF)
                    for c in range(8):
                        p.tr(bYv[:, c * 128:(c + 1) * 128], ybt[:, c * 128:(c + 1) * 128], ident_b.v)
                    p.copy("act", yT[:, 8:16, (ti - 1) * 128: ti * 128], bYv.rearrange("p (c t) -> p c t", t=128))

        if stop_after == "C2":
            p.dma("sp", yTd.v, yT.v.rearrange("p c t -> p (c t)"))
            p.barrier()
            return nc, dbg

        with p.scope() as sd:
            hts = [p.sb(sd, "hts%d" % i, [128, D], F32) for i in range(2)]
            zb1 = p.sb(sd, "zb1", [128, D], BF)
            junkd = p.sb(sd, "junkd", [128, D], BF)
            std = p.sb(sd, "std", [128, 8], F32)
            junkd2 = p.sb(sd, "junkd2", [128, D], BF)
            std2 = p.sb(sd, "std2", [128, 8], F32)

            def d_mm(t):
                p.dma("sp", hts[t % 2].v, hd[t * 128:(t + 1) * 128, :])
                for j in range(4):
                    bk = banks[(t % 2) * 4 + j]
                    for c in range(16):
                        p.mm(bk.v, yT[:, c, t * 128:(t + 1) * 128], wout[:, c, j * 512:(j + 1) * 512],
                             start=(c == 0), stop=(c == 15))

            def d_res(t):
                ht = hts[t % 2]
                for j in range(4):
                    p.stt("dve", ht[:, j * 512:(j + 1) * 512], ht[:, j * 512:(j + 1) * 512], ALPHA,
                          banks[(t % 2) * 4 + j].v, ALU.mult, ALU.add)

            def d_ln(t):
                ht = hts[t % 2]
                layer_norm(128, ht.v, zb1.v, gB1.v, bB1.v, std if t % 2 else std2, junkd.v if t % 2 else junkd2.v)
                p.dma("act", h1d[t * 128:(t + 1) * 128, :], ht.v)
                for half in range(2):
                    bk = banks[(t % 2) * 4 + half]; bv = bk.v.bitcast(BF)
                    for c in range(8):
                        cc = half * 8 + c
                        p.tr(bv[:, c * 128:(c + 1) * 128], zb1[:, cc * 128:(cc + 1) * 128], ident_b.v)
                    p.copy("act", yT[:, half * 8:(half + 1) * 8, t * 128:(t + 1) * 128],
                           bv.rearrange("p (c t) -> p c t", t=128))

            d_mm(0)
            d_res(0)
            for t in range(NT):
                if t + 1 < NT:
                    d_mm(t + 1)
                d_ln(t)
                if t + 1 < NT:
                    d_res(t + 1)
        p.barrier()
        p.release(*sW.tiles)
        sW.close()
        if stop_after == "D":
            p.barrier()
            return nc, dbg

        h1T = yT
        sD = scratch("sD", [S, 2048], F32)
        with p.scope() as s1_:
            qhT = p.sb(s1_, "qhT", [128, 16, S], BF)
            skn = p.sb(s1_, "skn", [128, 16, 128], BF)
            skT = p.sb(s1_, "skT", [128, 16, 128], BF)
            p.dma("pool", skn.v, V(sub_keys, sub_keys.h.ap().rearrange("a n c -> n a c")))
            for g in range(2):
                bk = bank(); bv = bk.v.bitcast(BF)
                for j in range(8):
                    p.tr(bv[:, j * 128:(j + 1) * 128], skn[:, g * 8 + j, :], ident_b.v)
                p.copy("act", skT[:, g * 8:(g + 1) * 8, :].rearrange("p a n -> p (a n)"), bv)
            wps = [p.sb(s1_, "wps%d" % i, [128, 16, 128], BF) for i in range(3)]
            w_pq_r = w_pq.h.ap().rearrange("(c p) n -> p c n", p=128)
            for hs in range(16):
                w = wps[hs % 3]
                p.dma("pool", w.v, V(w_pq, w_pq_r[:, :, hs * 128:(hs + 1) * 128]))
                for tb in range(4):
                    bk = bank()
                    for c in range(16):
                        p.mm(bk.v, w[:, c, :], h1T[:, c, tb * 512:(tb + 1) * 512], start=(c == 0), stop=(c == 15))
                    if (hs + tb) % 2:
                        p.copy("act", qhT[:, hs, tb * 512:(tb + 1) * 512], bk.v)
                    else:
                        p.copy("dve", qhT[:, hs, tb * 512:(tb + 1) * 512], bk.v)
            sst = [p.sb(s1_, "sst%d" % i, [128, 2048], F32) for i in range(2)]
            for t in range(NT):
                ss_ = sst[t % 2]
                for g in range(4):
                    bk = bank()
                    for j in range(4):
                        hs = g * 4 + j
                        p.mm(bk[:, j * 128:(j + 1) * 128], qhT[:, hs, t * 128:(t + 1) * 128], skT[:, hs, :])
                    if g % 2:
                        p.copy("act", ss_[:, g * 512:(g + 1) * 512], bk.v)
                    else:
                        p.copy("dve", ss_[:, g * 512:(g + 1) * 512], bk.v)
                p.dma("sp", sD[t * 128:(t + 1) * 128, :], ss_.v)
        if stop_after == "P1":
            p.barrier()
            return nc, dbg
        h1Td = scratch("h1Td", [128, 16 * S], BF)
        p.dma("sp", h1Td.v, yT.v.rearrange("p c t -> p (c t)"))
        p.barrier()
        p.release(yT)
        sY.close()
        GT3 = scratch("GT3", [NT, 128, 128 * 128], BF)
        with p.scope() as s2_:
            sts = [p.sb(s2_, "sts%d" % i, [128, 16, 128], F32) for i in range(2)]
            wa = p.sb(s2_, "wa", [128, 128], F32)
            t16 = p.sb(s2_, "t16", [128, 8, 2, 16], F32)
            cand = p.sb(s2_, "cand", [128, 256], F32)
            cw = p.sb(s2_, "cw", [128, 256], F32)
            c16 = p.sb(s2_, "c16", [128, 8, 16], F32)
            exs = p.sb(s2_, "exs", [128, 8, 16], F32)
            sm = p.sb(s2_, "sm", [128, 4, 8], F32)
            theta = p.sb(s2_, "theta", [128, 8, 16], F32)
            E_ = p.sb(s2_, "E_", [128, 16, 128], F32)
            OAfs = [p.sb(s2_, "OAf%d" % i, [128, 128, 64], BF) for i in range(2)]
            OBfs = [p.sb(s2_, "OBf%d" % i, [128, 128, 64], BF) for i in range(2)]
            OAp = p.sb(s2_, "OAp", [128, 128, 128], BF)
            OBp = p.sb(s2_, "OBp", [128, 128, 128], BF)
            Gs = p.sb(s2_, "Gs", [128, 128, 128], BF)
            ev = [0]
            for t in range(NT):
                st_ = sts[t % 2]
                if t == 0:
                    p.dma("sp", st_.v.rearrange("p a n -> p (a n)"), sD[0:128, :])
                if t + 1 < NT:
                    p.dma("sp", sts[(t + 1) % 2].v.rearrange("p a n -> p (a n)"), sD[(t + 1) * 128:(t + 2) * 128, :])
                st4 = st_.v.rearrange("p (h s) n -> p h s n", s=2)
                for hh in range(8):
                    for sd_ in range(2):
                        sv = st_[:, hh * 2 + sd_, :]
                        p.max8(t16[:, hh, sd_, 0:8], sv)
                        p.mrep(wa.v, t16[:, hh, sd_, 0:8], sv, -3.0e38)
                        p.max8(t16[:, hh, sd_, 8:16], wa.v)
                    cv = cand.v.rearrange("p (a b) -> p a b", b=16)
                    in0 = V(t16, t16.h[:, hh, 0, :].unsqueeze(2).to_broadcast([128, 16, 16]))
                    in1 = V(t16, t16.h[:, hh, 1, :].unsqueeze(1).to_broadcast([128, 16, 16]))
                    p.tt("dve", cv, in0, in1, ALU.add)
                    p.max8(c16[:, hh, 0:8], cand.v)
                    p.mrep(cw.v, c16[:, hh, 0:8], cand.v, -3.0e38)
                    p.max8(c16[:, hh, 8:16], cw.v)
                p.tt("dve", exs.v, c16.v, V(c16, c16.h[:, :, 0:1].to_broadcast([128, 8, 16])), ALU.subtract)
                p.act(exs.v, exs.v, AF.Exp)
                p.rsum("dve", sm[:, 0, :], exs.v)
                p.recip(sm[:, 1, :], sm[:, 0, :])
                p.ts("dve", sm[:, 2, :], c16[:, :, 15], -2.0e-5, None, ALU.add)
                p.tt("dve", theta.v, V(sm, sm.h[:, 2, :].unsqueeze(2).to_broadcast([128, 8, 16])), t16[:, :, 0, :],
                     ALU.subtract)
                m16 = t16.h[:, :, :, 0:1].rearrange("p h s o -> p (h s) o").to_broadcast([128, 16, 128])
                p.tt("dve", E_.v, st_.v, V(t16, m16), ALU.subtract)
                p.act(E_.v, E_.v, AF.Exp)
                E4 = E_.v.rearrange("p (h s) n -> p h s n", s=2)
                p.tt("dve", E4[:, :, 0, :], E4[:, :, 0, :],
                     V(sm, sm.h[:, 1, :].unsqueeze(2).to_broadcast([128, 8, 128])), ALU.mult)
                for hf_ in range(2):
                    OAf = OAfs[hf_]; OBf = OBfs[hf_]
                    isl = slice(hf_ * 64, (hf_ + 1) * 64)
                    for qd in range(4):
                        hs_ = slice(qd * 2, qd * 2 + 2)
                        OA = V(OAf, OAf.h[:, qd * 32:(qd + 1) * 32, :].rearrange("t (h a) i -> t h a i", h=2))
                        OB = V(OBf, OBf.h[:, qd * 32:(qd + 1) * 32, :].rearrange("t (h a) i -> t h a i", h=2))
                        sa_b = V(st_, st4.ap[:, hs_, 0, isl].unsqueeze(2).to_broadcast([128, 2, 16, 64]))
                        sb_b = V(st_, st4.ap[:, hs_, 1, isl].unsqueeze(2).to_broadcast([128, 2, 16, 64]))
                        s1_b = V(t16, t16.h[:, hs_, 0, :].unsqueeze(3).to_broadcast([128, 2, 16, 64]))
                        th_b = V(theta, theta.h[:, hs_, :].unsqueeze(3).to_broadcast([128, 2, 16, 64]))
                        ea_b = V(E_, E4.ap[:, hs_, 0, isl].unsqueeze(2).to_broadcast([128, 2, 16, 64]))
                        eb_b = V(E_, E4.ap[:, hs_, 1, isl].unsqueeze(2).to_broadcast([128, 2, 16, 64]))
                        p.tt("dve", OA, sa_b, s1_b, ALU.is_equal)
                        p.tt("dve", OA, OA, ea_b, ALU.mult)
                        p.tt("dve", OB, sb_b, th_b, ALU.is_ge)
                        p.tt("dve", OB, OB, eb_b, ALU.mult)
                    for (src, dstp) in ((OAf, OAp), (OBf, OBp)):
                        for ig in range(8):
                            bk = bank(); bv = bk.v.bitcast(BF)
                            for j in range(8):
                                p.tr(bv[:, j * 128:(j + 1) * 128], src[:, :, ig * 8 + j], ident_b.v)
                            i0_ = hf_ * 64 + ig * 8
                            p.copy("act", dstp[:, i0_:i0_ + 8, :].rearrange("p i t -> p (i t)"), bv)
                for tl in range(128):
                    if tl % 4 == 0:
                        bk = bank()
                        bkv = bk.v.rearrange("p (j f) -> p j f", f=4)
                    p.mm(bkv[:, :, tl % 4], OAp[:, :, tl], OBp[:, :, tl])
                    if tl % 4 == 3:
                        ev[0] += 1
                        p.copy("act", Gs[:, :, tl - 3:tl + 1], bkv)
                p.dma("act", GT3[t], Gs.v.rearrange("p j t -> p (j t)"))
        if stop_after == "P2":
            p.barrier()
            return nc, dbg
        IG = 4
        u_r = u_tab.h.ap().rearrange("(i j) d -> j i d", j=128)
        v_r = v_tab.h.ap().rearrange("(i j) d -> j i d", j=128)
        sH = contextlib.ExitStack()
        sH.tiles = []
        es.enter_context(sH)
        h1T = p.sb(sH, "h1T", [128, 16, S], BF)
        p.dma("sp", h1T.v.rearrange("p c t -> p (c t)"), h1Td.v)
        for hf in range(2):
            with p.scope() as s3_:
                acc = p.sb(s3_, "acc", [128, 8, D], F32)
                with p.scope() as s4_:
                    urows = [p.sb(s4_, "urow%d" % i, [128, D], BF) for i in range(3)]
                    uTs = [p.sb(s4_, "uT%d" % i, [128, 16, 128], BF) for i in range(2)]
                    gti = [p.sb(s4_, "gti%d" % i, [128, 1024], BF) for i in range(2)]
                    gel = [p.sb(s4_, "gel%d" % i, [128, 512], BF) for i in range(2)]
                    GHs = [p.sb(s4_, "GH%d" % i, [128, IG, 1024], BF) for i in range(2)]
                    vss = [p.sb(s4_, "vs%d" % i, [128, IG, D], BF) for i in range(2)]
                    gi = [0]

                    def load(i_):
                        ur = urows[i_ % 3]
                        vs = vss[(i_ // IG) % 2]
                        p.dma("pool", ur.v, V(u_tab, u_r[i_]))
                        p.dma("pool", vs[:, i_ % IG, :], V(v_tab, v_r[i_]))

                    def prep(i_):
                        ur = urows[i_ % 3]; uT = uTs[i_ % 2]; gt = gti[i_ % 2]
                        src_g = GT3.h[hf * 8:(hf + 1) * 8, :, i_ * 128:(i_ + 1) * 128].rearrange("a i t -> i a t")
                        p.dma("sp", gt.v.rearrange("p (a t) -> p a t", t=128), V(GT3, src_g))
                        for g in range(2):
                            bk = bank(); bv = bk.v.bitcast(BF)
                            for j in range(8):
                                c = g * 8 + j
                                p.tr(bv[:, j * 128:(j + 1) * 128], ur[:, c * 128:(c + 1) * 128], ident_b.v)
                            p.copy("act", uT[:, g * 8:(g + 1) * 8, :].rearrange("p c j -> p (c j)"), bv)

                    def hidden(i_):
                        uT = uTs[i_ % 2]; gt = gti[i_ % 2]
                        GH = GHs[(i_ // IG) % 2]; il = i_ % IG
                        for tb in range(2):
                            t0 = hf * 1024 + tb * 512
                            bk = bank()
                            for c in range(16):
                                p.mm(bk.v, uT[:, c, :], h1T[:, c, t0:t0 + 512], start=(c == 0), stop=(c == 15))
                            ge = gel[gi[0] % 2]; gi[0] += 1
                            p.act(ge.v, bk.v, AF.Gelu)
                            p.tt("dve", GH[:, il, tb * 512:(tb + 1) * 512], ge.v, gt[:, tb * 512:(tb + 1) * 512],
                                 ALU.mult)

                    def outmm(ig, tt_):
                        GH = GHs[ig % 2]; vs = vss[ig % 2]
                        bks = [bank() for _ in range(4)]
                        for j in range(4):
                            for il in range(IG):
                                p.mm(bks[j].v, GH[:, il, tt_ * 128:(tt_ + 1) * 128], vs[:, il, j * 512:(j + 1) * 512],
                                     start=(il == 0), stop=(il == IG - 1))
                        for j in range(4):
                            a_ = acc[:, tt_, j * 512:(j + 1) * 512]
                            if ig == 0:
                                p.copy("dve", a_, bks[j].v)
                            else:
                                p.tt("dve", a_, a_, bks[j].v, ALU.add)

                    sched = {0: [0, 1, 2], 1: [3, 4, 5], 2: [6, 7], 3: []}
                    load(0)
                    load(1)
                    prep(0)
                    for i_ in range(128):
                        ig = i_ // IG
                        if i_ + 1 < 128:
                            prep(i_ + 1)
                        hidden(i_)
                        if ig >= 1:
                            for k in sched[i_ % IG]:
                                outmm(ig - 1, k)
                        if i_ + 2 < 128:
                            load(i_ + 2)
                    for tt_ in range(8):
                        outmm(128 // IG - 1, tt_)
                with p.scope() as s5_:
                    gB2 = p.sb(s5_, "gB2", [128, D], F32)
                    bB2 = p.sb(s5_, "bB2", [128, D], F32)
                    p.dma("sp", gB2.v, V(ln2_g, ln2_g.h.ap().partition_broadcast(128)))
                    p.dma("sp", bB2.v, V(ln2_b, ln2_b.h.ap().partition_broadcast(128)))
                    h1s = [p.sb(s5_, "h1s%d" % i, [128, D], F32) for i in range(2)]
                    ots = [p.sb(s5_, "ots%d" % i, [128, D], F32) for i in range(2)]
                    junk2s = [p.sb(s5_, "junk2%d" % i, [128, D], BF) for i in range(2)]
                    st2s = [p.sb(s5_, "st2%d" % i, [128, 8], F32) for i in range(2)]
                    for tt_ in range(8):
                        t = hf * 8 + tt_
                        h1 = h1s[tt_ % 2]; ot = ots[tt_ % 2]
                        p.dma("sp", h1.v, h1d[t * 128:(t + 1) * 128, :])
                        p.stt("dve", h1.v, h1.v, ALPHA, acc[:, tt_, :], ALU.mult, ALU.add)
                        layer_norm(128, h1.v, ot.v, gB2.v, bB2.v, st2s[tt_ % 2], junk2s[tt_ % 2].v)
                        p.dma("act", out_d[t * 128:(t + 1) * 128, :], ot.v)


        p.barrier()
    return nc, dbg


def make_consts():
    ident = np.eye(128, dtype=np.float32)
    tri = np.triu(np.ones((128, 128), dtype=np.float32))
    boh = np.zeros((32, 512), dtype=np.float32)
    rlt = [[15, 165], [14, 27], [13, 18], [12, 14], [11, 9], [10, 7], [9, 4], [8, 4], [7, 1], [6, 1], [5, 1],
           [4, 1], [3, 1], [2, 1], [1, 1], [0, 1], [17, 1], [18, 1], [19, 1], [20, 1], [21, 1], [22, 1], [23, 1],
           [24, 4], [25, 4], [26, 7], [27, 9], [28, 14], [29, 18], [30, 27], [31, 37]]
    bucket = []
    for v, n in rlt:
        bucket += [v] * n
    for i in range(383):
        boh[bucket[i], i] = 1.0
    jrev = np.ascontiguousarray(np.eye(128, dtype=np.float32)[::-1])
    return {"c_ident": ident, "c_tri": tri, "c_boh": boh, "c_jrev": jrev}


def core_inputs(inputs, b):
    m = {
        "x": np.ascontiguousarray(inputs["x"][b]),
        "meta_tokens": np.ascontiguousarray(inputs["meta_tokens"]),
        "ln0_g": inputs["ln0_g"].reshape(1, D), "ln0_b": inputs["ln0_b"].reshape(1, D),
        "rel_bias": np.ascontiguousarray(inputs["rel_bias"]),
        "w_in": np.ascontiguousarray(inputs["w_in"][0]),
        "w_uk": np.ascontiguousarray(inputs["w_uk"][0]), "w_uv": np.ascontiguousarray(inputs["w_uv"][0]),
        "w_gk2": np.ascontiguousarray(inputs["w_gk2"][0]), "b_gk": inputs["b_gk"].reshape(1, 512),
        "gla_norm_g": inputs["gla_norm_g"].reshape(1, 256),
        "w_out": np.ascontiguousarray(inputs["w_out"][0]),
        "ln1_g": inputs["ln1_g"].reshape(1, D), "ln1_b": inputs["ln1_b"].reshape(1, D),
        "w_pq": np.ascontiguousarray(inputs["w_pq"][0]),
        "sub_keys": np.ascontiguousarray(inputs["sub_keys"][0]).reshape(16, 128, 128),
        "u_tab": np.ascontiguousarray(inputs["u_tab"][0]), "v_tab": np.ascontiguousarray(inputs["v_tab"][0]),
        "ln2_g": inputs["ln2_g"].reshape(1, D), "ln2_b": inputs["ln2_b"].reshape(1, D),
    }
    m.update(make_consts())
    return {k: np.asarray(v, dtype=np.float32) for k, v in m.items()}


def kernel(**inputs):
    inputs = {k: np.asarray(v) for k, v in inputs.items()}
    nc, _ = build()
    in_maps = [core_inputs(inputs, b) for b in range(8)]
    res = run_bass_kernel_spmd(nc, in_maps, core_ids=list(range(8)))
    return np.stack([np.asarray(r["out"]) for r in res.results], axis=0).astype(np.float32)
```
